# Optimizing a Trainium2 kernel written in Bass

```python
import jax, jax.numpy as jnp
from jax import lax
import numpy as np

D_MODEL = 1024
BATCH = 8
SEQ = 2048
DEPTH = 2

MIX_WIDTH = D_MODEL
SGU_WIDTH = MIX_WIDTH // 2
SGU_GROUPS = 4
SGU_GROUP_DIM = SGU_WIDTH // SGU_GROUPS
CHUNK = 128
SB_WIDTH = MIX_WIDTH - SGU_WIDTH
SB_HEADS = 8
SB_HEAD_DIM = SB_WIDTH // SB_HEADS
Q_BLOCK = 128
IN_COLS = 2 * SGU_WIDTH + 3 * SB_WIDTH
MEM_LEN = 256
MEM_HEADS = 4
MEM_HEAD_DIM = D_MODEL // MEM_HEADS
N_EXPERTS = 32
TOP_K = 4
D_EXPERT = D_MODEL
SWIGLU_LIMIT = 7.0
SWIGLU_ALPHA = 1.702
MOE_BLOCK = 128
DEEPNORM_ALPHA = (2 * DEPTH) ** 0.25
DEEPNORM_BETA = (8 * DEPTH) ** -0.25
LN_EPS = 1e-5
RMS_EPS = 1e-6

kernel_name = 'hybrid_sgu_stickbreak_mem_moe_deepnorm'


def layer_norm(x, g, b):
    xf = x.astype(jnp.float32)
    mu = jnp.mean(xf, axis=-1, keepdims=True)
    var = jnp.mean(jnp.square(xf - mu), axis=-1, keepdims=True)
    y = (xf - mu) * lax.rsqrt(var + LN_EPS)
    return (y * g.astype(jnp.float32) + b.astype(jnp.float32)).astype(x.dtype)


def group_layer_norm(x, g, b, groups):
    shp = x.shape
    xf = x.astype(jnp.float32).reshape(shp[:-1] + (groups, shp[-1] // groups))
    mu = jnp.mean(xf, axis=-1, keepdims=True)
    var = jnp.mean(jnp.square(xf - mu), axis=-1, keepdims=True)
    y = ((xf - mu) * lax.rsqrt(var + LN_EPS)).reshape(shp)
    return (y * g.astype(jnp.float32) + b.astype(jnp.float32)).astype(x.dtype)


def group_rms(x, groups):
    shp = x.shape
    xf = x.astype(jnp.float32).reshape(shp[:-1] + (groups, shp[-1] // groups))
    y = xf * lax.rsqrt(jnp.mean(jnp.square(xf), axis=-1, keepdims=True) + RMS_EPS)
    return y.reshape(shp).astype(x.dtype)


def stick_breaking_attention(q, k, v):
    S = q.shape[2]
    scale = SB_HEAD_DIM ** -0.5
    qf = q.astype(jnp.float32)
    kf = k.astype(jnp.float32)
    outs = []
    for i in range(S // Q_BLOCK):
        lo, hi = i * Q_BLOCK, (i + 1) * Q_BLOCK
        z = jnp.einsum('bhtd,bhsd->bhts', qf[:, :, lo:hi], kf[:, :, :hi]) * scale
        t_pos = lo + jnp.arange(Q_BLOCK)[:, None]
        s_pos = jnp.arange(hi)[None, :]
        mask = s_pos < t_pos
        log_not = jnp.where(mask, jax.nn.log_sigmoid(-z), 0.0)
        log_w = jax.nn.log_sigmoid(z) + lax.cumsum(log_not, axis=3, reverse=True) - log_not
        a = jnp.where(mask, jnp.exp(log_w), 0.0)
        outs.append(jnp.einsum('bhts,bhsd->bhtd', a.astype(v.dtype), v[:, :, :hi]))
    return jnp.concatenate(outs, axis=2)


def parallel_mixer(h, w_in, sgu_g, sgu_b, w_sp, b_sp, grp_g, w_out):
    B, S, _ = h.shape
    proj = h @ w_in
    u, v_s, q, k, v = jnp.split(
        proj, [SGU_WIDTH, 2 * SGU_WIDTH, 2 * SGU_WIDTH + SB_WIDTH, 2 * SGU_WIDTH + 2 * SB_WIDTH], axis=-1)
    u = jax.nn.gelu(u)
    v_s = group_layer_norm(jax.nn.gelu(v_s), sgu_g, sgu_b, SGU_GROUPS)
    vc = v_s.reshape(B, S // CHUNK, CHUNK, SGU_GROUPS, SGU_GROUP_DIM)
    causal = jnp.tril(jnp.ones((CHUNK, CHUNK), dtype=bool))
    w_c = jnp.where(causal[None], w_sp, 0.0).astype(vc.dtype)
    sg = jnp.einsum('gts,bcsgd->bctgd', w_c, vc) + b_sp.T[:, :, None]
    out_a = u * sg.reshape(B, S, SGU_WIDTH)
    to_heads = lambda t: t.reshape(B, S, SB_HEADS, SB_HEAD_DIM).transpose(0, 2, 1, 3)
    out_b = stick_breaking_attention(to_heads(q), to_heads(k), to_heads(v))
    out_b = out_b.transpose(0, 2, 1, 3).reshape(B, S, SB_WIDTH)
    y = jnp.concatenate([group_rms(out_a, SGU_GROUPS), group_rms(out_b, SB_HEADS)], axis=-1) * grp_g
    return y @ w_out


def memory_attention(x, mem, wq, wkv, wo):
    B, S, D = x.shape
    M = mem.shape[1]
    q = (x @ wq).reshape(B, S, MEM_HEADS, MEM_HEAD_DIM)
    k, v = jnp.split(mem @ wkv, 2, axis=-1)
    k = k.reshape(B, M, MEM_HEADS, MEM_HEAD_DIM)
    v = v.reshape(B, M, MEM_HEADS, MEM_HEAD_DIM)
    s = jnp.einsum('bshd,bmhd->bhsm', q.astype(jnp.float32), k.astype(jnp.float32)) * (MEM_HEAD_DIM ** -0.5)
    p = jax.nn.softmax(s, axis=-1).astype(x.dtype)
    o = jnp.einsum('bhsm,bmhd->bshd', p, v).reshape(B, S, D)
    return o @ wo


def moe(x, w_router, b_router, w_gu, b_gu, w_down, b_down):
    B, S, D = x.shape
    N = B * S
    NK = N * TOP_K
    x2 = x.reshape(N, D)
    logits = (x2 @ w_router + b_router).astype(jnp.float32)
    top_val, top_idx = lax.top_k(logits, TOP_K)
    gates = jax.nn.softmax(top_val, axis=-1).astype(x.dtype)
    flat_e = top_idx.reshape(NK).astype(jnp.int32)
    flat_tok = jnp.arange(NK, dtype=jnp.int32) // TOP_K
    order = jnp.argsort(flat_e)
    e_sorted = flat_e[order]
    tok_sorted = flat_tok[order]
    gate_sorted = gates.reshape(NK)[order]
    counts = jnp.bincount(flat_e, length=N_EXPERTS)
    starts = jnp.cumsum(counts) - counts
    padded = ((counts + MOE_BLOCK - 1) // MOE_BLOCK) * MOE_BLOCK
    pend = jnp.cumsum(padded)
    pstart = pend - padded
    dest = pstart[e_sorted] + jnp.arange(NK, dtype=jnp.int32) - starts[e_sorted]
    P = ((NK + MOE_BLOCK - 1) // MOE_BLOCK) * MOE_BLOCK + N_EXPERTS * MOE_BLOCK
    n_blocks = P // MOE_BLOCK
    row_tok = jnp.full((P,), N, dtype=jnp.int32).at[dest].set(tok_sorted)
    x_pad = jnp.concatenate([x2, jnp.zeros((1, D), x2.dtype)], axis=0)
    rows = x_pad[row_tok].reshape(n_blocks, MOE_BLOCK, D)
    block_e = jnp.searchsorted(pend, jnp.arange(n_blocks) * MOE_BLOCK, side='right')
    block_e = jnp.clip(block_e, 0, N_EXPERTS - 1)

    def expert_block(args):
        xb, e = args
        hgu = xb @ w_gu[e] + b_gu[e]
        g, u = hgu[:, :D_EXPERT], hgu[:, D_EXPERT:]
        g = jnp.minimum(g, SWIGLU_LIMIT)
        u = jnp.clip(u, -SWIGLU_LIMIT, SWIGLU_LIMIT)
        a = (u + 1.0) * (g * jax.nn.sigmoid(SWIGLU_ALPHA * g))
        return a @ w_down[e] + b_down[e]

    y = lax.map(expert_block, (rows, block_e)).reshape(P, D)
    y_sel = y[dest] * gate_sorted[:, None]
    out = jax.ops.segment_sum(y_sel, tok_sorted, num_segments=N)
    return out.reshape(B, S, D)


def setup_inputs(seed: int = 0) -> dict:
    key = jax.random.key(seed)
    ks = jax.random.split(key, 24)
    f32 = jnp.float32
    nrm = lambda k, shp, s: jax.random.normal(k, shp, f32) * s
    L, D, E, F = DEPTH, D_MODEL, N_EXPERTS, D_EXPERT
    return {
        'x': nrm(ks[0], (BATCH, SEQ, D), 1.0),
        'mem': nrm(ks[1], (BATCH, MEM_LEN, D), 1.0),
        'w_in': nrm(ks[2], (L, D, IN_COLS), D ** -0.5),
        'sgu_g': 1.0 + nrm(ks[3], (L, SGU_WIDTH), 0.1),
        'sgu_b': nrm(ks[4], (L, SGU_WIDTH), 0.02),
        'w_sp': nrm(ks[5], (L, SGU_GROUPS, CHUNK, CHUNK), CHUNK ** -0.5),
        'b_sp': 1.0 + nrm(ks[6], (L, SGU_GROUPS, CHUNK), 0.1),
        'grp_g': 1.0 + nrm(ks[7], (L, MIX_WIDTH), 0.1),
        'w_out': nrm(ks[8], (L, MIX_WIDTH, D), DEEPNORM_BETA * MIX_WIDTH ** -0.5),
        'wq_mem': nrm(ks[9], (L, D, D), D ** -0.5),
        'wkv_mem': nrm(ks[10], (L, D, 2 * D), D ** -0.5),
        'wo_mem': nrm(ks[11], (L, D, D), DEEPNORM_BETA * D ** -0.5),
        'w_router': nrm(ks[12], (L, D, E), D ** -0.5),
        'b_router': nrm(ks[13], (L, E), 0.01),
        'w_gu': nrm(ks[14], (L, E, D, 2 * F), D ** -0.5),
        'b_gu': nrm(ks[15], (L, E, 2 * F), 0.02),
        'w_down': nrm(ks[16], (L, E, F, D), DEEPNORM_BETA * F ** -0.5),
        'b_down': nrm(ks[17], (L, E, D), 0.02),
        'ln_g': 1.0 + nrm(ks[18], (L, 3, D), 0.1),
        'ln_b': nrm(ks[19], (L, 3, D), 0.02),
    }


def reference(x, mem, w_in, sgu_g, sgu_b, w_sp, b_sp, grp_g, w_out, wq_mem, wkv_mem, wo_mem,
              w_router, b_router, w_gu, b_gu, w_down, b_down, ln_g, ln_b):
    for l in range(DEPTH):
        mix = parallel_mixer(x, w_in[l], sgu_g[l], sgu_b[l], w_sp[l], b_sp[l], grp_g[l], w_out[l])
        x = layer_norm(DEEPNORM_ALPHA * x + mix, ln_g[l, 0], ln_b[l, 0])
        xa = memory_attention(x, mem, wq_mem[l], wkv_mem[l], wo_mem[l])
        x = layer_norm(DEEPNORM_ALPHA * x + xa, ln_g[l, 1], ln_b[l, 1])
        ff = moe(x, w_router[l], b_router[l], w_gu[l], b_gu[l], w_down[l], b_down[l])
        x = layer_norm(DEEPNORM_ALPHA * x + ff, ln_g[l, 2], ln_b[l, 2])
    return x
```

```python
import contextlib
import numpy as np
import concourse.bass as bass
import concourse.mybir as mybir
from concourse.bass_utils import run_bass_kernel_spmd

F32 = mybir.dt.float32
BF16 = mybir.dt.bfloat16
I32 = mybir.dt.int32
AF = mybir.ActivationFunctionType
ALU = mybir.AluOpType

ENGS = ("pe", "act", "dve", "pool", "sp")
DEPTH = 2
ALPHA = float((2 * DEPTH) ** 0.25)
LN_EPS = 1e-5
RMS_EPS = 1e-6
SW_A = 1.702
SW_L = 7.0


class Buf:
    __slots__ = ("name", "last_w", "readers")

    def __init__(self, name=""):
        self.name = name
        self.last_w = None
        self.readers = []


class Op:
    __slots__ = ("eng", "fn", "is_dma", "pos", "waits", "dma_waits", "marked", "tick",
                 "dsem", "dval", "pre_dma_wait")

    def __init__(self, eng, fn, is_dma):
        self.eng = eng
        self.fn = fn
        self.is_dma = is_dma
        self.pos = -1
        self.waits = {}
        self.dma_waits = []
        self.marked = False
        self.tick = 0
        self.dsem = None
        self.dval = 0
        self.pre_dma_wait = None


class _Rec:
    def __init__(self):
        self.call = None

    def __getattr__(self, name):
        def f(*a, **k):
            self.call = (name, a, k)
            return self
        return f


class Prog:
    def __init__(self, nc, n_dma_sems=8):
        self.nc = nc
        self.ops = {e: [] for e in ENGS}
        self.seen = {e: {p: -1 for p in ENGS} for e in ENGS}
        self.seen_dma = {e: set() for e in ENGS}
        self.n_dma_sems = n_dma_sems
        self.dma_count = {e: 0 for e in ENGS}
        self.dma_hist = {e: [] for e in ENGS}
        self.pending_dmas = []
        self.all_dma_out = []

    def _add_dep(self, op, dep):
        if dep is None or dep is op:
            return
        e = op.eng
        if dep.is_dma:
            if dep in self.seen_dma[e]:
                return
            self.seen_dma[e].add(dep)
            op.dma_waits.append(dep)
            return
        if dep.eng == e and e == "pe" and not op.is_dma:
            return
        if self.seen[e][dep.eng] >= dep.pos:
            return
        cur = op.waits.get(dep.eng)
        if cur is None or cur.pos < dep.pos:
            op.waits[dep.eng] = dep

    def _finish(self, op):
        eng = op.eng
        for pe_, d in op.waits.items():
            self.seen[eng][pe_] = max(self.seen[eng][pe_], d.pos)
            d.marked = True
        self.ops[eng].append(op)

    def _record(self, eng, fn, reads, writes, is_dma):
        rec = _Rec()
        fn(rec)
        name_, a_, k_ = rec.call
        fn = (lambda e, name_=name_, a_=a_, k_=k_: getattr(e, name_)(*a_, **k_))
        op = Op(eng, fn, is_dma)
        op.pos = len(self.ops[eng])
        for b in reads:
            self._add_dep(op, b.last_w)
        for b in writes:
            self._add_dep(op, b.last_w)
            for r in b.readers:
                self._add_dep(op, r)
        for b in reads:
            b.readers.append(op)
        for b in writes:
            b.last_w = op
            b.readers = []
        if is_dma:
            k = self.dma_count[eng]
            self.dma_count[eng] += 1
            op.dsem = k % self.n_dma_sems
            op.dval = 16 * (k // self.n_dma_sems + 1)
            hist = self.dma_hist[eng]
            if k >= self.n_dma_sems:
                prev = hist[k - self.n_dma_sems]
                if prev not in self.seen_dma[eng]:
                    self.seen_dma[eng].add(prev)
                    op.pre_dma_wait = prev
            hist.append(op)
            self.pending_dmas.append(op)
        self._finish(op)
        return op

    def op(self, eng, fn, reads=(), writes=()):
        return self._record(eng, fn, reads, writes, False)

    def dma(self, eng, fn, reads=(), writes=(), is_output=False):
        o = self._record(eng, fn, reads, writes, True)
        if is_output:
            self.all_dma_out.append(o)
        return o

    def _last_compute(self, e):
        for o in reversed(self.ops[e]):
            if not o.is_dma and o.fn is not None:
                return o
        return None

    def barrier(self):
        scr = self.bar_scratch
        op = Op("dve", lambda e, scr=scr: e.memset(scr[0:1, 0:1], 0.0), False)
        op.pos = len(self.ops["dve"])
        for p in ENGS:
            d = self._last_compute(p)
            if d is not None:
                self._add_dep(op, d)
        for d in self.pending_dmas:
            self._add_dep(op, d)
        self.pending_dmas = []
        self._finish(op)
        for e in ENGS:
            if e == "dve":
                continue
            w = Op(e, None, False)
            w.pos = len(self.ops[e])
            self._add_dep(w, op)
            self._finish(w)

    def emit(self):
        nc = self.nc
        engmap = {"pe": "tensor", "act": "scalar", "dve": "vector", "pool": "gpsimd", "sp": "sync"}
        final = Op("sp", None, False)
        final.pos = len(self.ops["sp"])
        for d in self.all_dma_out:
            if d not in self.seen_dma["sp"]:
                final.dma_waits.append(d)
        for e in ENGS:
            t = 0
            for o in self.ops[e]:
                if o.marked and not o.is_dma:
                    t += 1
                o.tick = t
        with contextlib.ExitStack() as st:
            sems = {e: st.enter_context(nc.semaphore("s_" + e)) for e in ENGS}
            dsems = {e: [st.enter_context(nc.semaphore("d_%s_%d" % (e, i)))
                         for i in range(self.n_dma_sems)] for e in ("sp", "act", "pool")}
            block = st.enter_context(nc.Block())

            def make(e):
                def body(eng):
                    lst = list(self.ops[e])
                    if e == "sp":
                        lst = lst + [final]
                    for o in lst:
                        for pe_, d in o.waits.items():
                            eng.wait_ge(sems[pe_], d.tick)
                        for d in o.dma_waits:
                            eng.wait_ge(dsems[d.eng][d.dsem], d.dval)
                        if o.pre_dma_wait is not None:
                            d = o.pre_dma_wait
                            eng.wait_ge(dsems[d.eng][d.dsem], d.dval)
                        if o.fn is None:
                            continue
                        ins = o.fn(eng)
                        if o.is_dma:
                            ins.then_inc(dsems[e][o.dsem], 16)
                        elif o.marked:
                            ins.then_inc(sems[e], 1)
                return body

            for e in ENGS:
                getattr(block, engmap[e])(make(e))


class T:
    def __init__(self, t, nb=1):
        self.t = t
        self.b = [Buf() for _ in range(nb)]


W_NAMES = ["w_in", "sgu_g", "sgu_b", "w_sp", "b_sp", "grp_g", "w_out", "wq_mem", "wkv_mem", "wo_mem",
           "w_router", "b_router", "w_gu", "b_gu", "w_down", "b_down", "ln_g", "ln_b"]
W_SHAPES = {
    "w_in": [2, 1024, 2560], "sgu_g": [2, 512], "sgu_b": [2, 512], "w_sp": [2, 4, 128, 128],
    "b_sp": [2, 4, 128], "grp_g": [2, 1024], "w_out": [2, 1024, 1024], "wq_mem": [2, 1024, 1024],
    "wkv_mem": [2, 1024, 2048], "wo_mem": [2, 1024, 1024], "w_router": [2, 1024, 32],
    "b_router": [2, 32], "w_gu": [2, 32, 1024, 2048], "b_gu": [2, 32, 2048],
    "w_down": [2, 32, 1024, 1024], "b_down": [2, 32, 1024], "ln_g": [2, 3, 1024], "ln_b": [2, 3, 1024],
}


def build(stop=None, n_layers=DEPTH, n_experts=32):
    nc = bass.Bass("TRN2", target_bir_lowering=False)
    P = Prog(nc)
    D = {}
    D["x"] = nc.dram_tensor("x", [2048, 1024], F32, kind="ExternalInput").ap()
    D["mem"] = nc.dram_tensor("mem", [256, 1024], F32, kind="ExternalInput").ap()
    for n in W_NAMES:
        D[n] = nc.dram_tensor(n, W_SHAPES[n], F32, kind="ExternalInput").ap()
    out_d = nc.dram_tensor("out", [2048, 1024], F32, kind="ExternalOutput").ap()

    rr = [0]
    uid = [0]

    with contextlib.ExitStack() as top:
        def _sp():
            try:
                rr[0] += 1000
                nc.sbuf_tensor("zzprobe%d" % rr[0], [128, 60000], F32).__enter__()
            except BaseException as ex:
                import re as _re
                m_ = _re.search(r"base=(\d+)", str(ex))
                return int(m_.group(1)) if m_ else -1
            return -2

        def sb(st, name, shape, dt=F32, nb=1):
            import os as _os
            if _os.environ.get("ALLOCDBG"):
                b0 = _sp()
                t_ = T(st.enter_context(nc.sbuf_tensor(name, shape, dt)), nb)
                print("ALLOC", name, b0, _sp())
                return t_
            uid[0] += 1
            return T(st.enter_context(nc.sbuf_tensor("%s_%d" % (name, uid[0]), shape, dt)), nb)

        X = sb(top, "X", [128, 16, 1024], F32, nb=16)
        ps = [T(top.enter_context(nc.psum_tensor("ps%d" % i, [128, 512], F32))) for i in range(8)]
        P.bar_scratch = top.enter_context(nc.sbuf_tensor("barscr", [128, 8], F32))
        ident = sb(top, "ident", [128, 128])
        tmat = sb(top, "tmat", [128, 128], BF16)
        ones_b = sb(top, "ones_b", [128, 128], BF16)
        nones_b = sb(top, "nones_b", [128, 128], BF16)
        ntmat = sb(top, "ntmat", [128, 128], BF16)
        bd_f = sb(top, "bd_f", [128, 128])
        cmask = sb(top, "cmask", [128, 128])
        m01 = sb(top, "m01", [128, 896], BF16)
        mneg = sb(top, "mneg", [128, 896])
        umat = sb(top, "umat", [128, 128], BF16)
        iosl = sb(top, "iosl", [128, 128])
        cst = contextlib.ExitStack()
        iof = sb(cst, "iof", [128, 896])
        iop = sb(cst, "iop", [128, 1])
        ioi = sb(cst, "ioi", [128, 896], I32)
        iopi = sb(cst, "iopi", [128, 1], I32)

        def cp_engine():
            rr[0] += 1
            return "act" if rr[0] % 2 else "dve"

        def copy(eng, out, in_, reads, writes, scale=None):
            if eng == "act":
                if scale is None:
                    P.op("act", lambda e: e.activation(out=out, in_=in_, func=AF.Copy), reads, writes)
                else:
                    P.op("act", lambda e: e.activation(out=out, in_=in_, func=AF.Copy, scale=scale), reads, writes)
            else:
                if scale is None:
                    P.op(eng, lambda e: e.tensor_copy(out=out, in_=in_), reads, writes)
                else:
                    P.op(eng, lambda e: e.tensor_scalar(out=out, in0=in_, scalar1=scale, scalar2=None, op0=ALU.mult), reads, writes)

        P.op("pool", lambda e: e.iota(ioi.t[:], [[1, 896]], base=-384, channel_multiplier=0), writes=ioi.b)
        P.op("pool", lambda e: e.iota(iopi.t[:], [[0, 1]], base=0, channel_multiplier=1), writes=iopi.b)
        P.op("dve", lambda e: e.tensor_copy(out=iof.t[:], in_=ioi.t[:]), ioi.b, iof.b)
        P.op("dve", lambda e: e.tensor_copy(out=iop.t[:], in_=iopi.t[:]), iopi.b, iop.b)
        jf = iof.t[:, 384:512]
        cb = iof.b + iop.b
        P.op("dve", lambda e: e.tensor_scalar(out=ident.t[:], in0=jf, scalar1=iop.t[:, 0:1], scalar2=None, op0=ALU.is_equal), cb, ident.b)
        P.op("dve", lambda e: e.tensor_scalar(out=tmat.t[:], in0=jf, scalar1=iop.t[:, 0:1], scalar2=None, op0=ALU.is_lt), cb, tmat.b)
        P.op("dve", lambda e: e.tensor_scalar(out=cmask.t[:], in0=jf, scalar1=iop.t[:, 0:1], scalar2=None, op0=ALU.is_ge), cb, cmask.b)
        P.op("dve", lambda e: e.tensor_scalar(out=m01.t[:], in0=iof.t[:], scalar1=iop.t[:, 0:1], scalar2=None, op0=ALU.is_gt), cb, m01.b)
        P.op("dve", lambda e: e.tensor_scalar(out=mneg.t[:], in0=iof.t[:], scalar1=iop.t[:, 0:1], scalar2=None, op0=ALU.is_gt), cb, mneg.b)
        P.op("dve", lambda e: e.tensor_scalar(out=mneg.t[:], in0=mneg.t[:], scalar1=1.0, scalar2=30000.0, op0=ALU.subtract, op1=ALU.mult), mneg.b, mneg.b)
        P.op("pool", lambda e: e.memset(ones_b.t[:], 1.0), writes=ones_b.b)
        P.op("pool", lambda e: e.memset(nones_b.t[:], -1.0), writes=nones_b.b)
        P.op("dve", lambda e: e.tensor_scalar(out=ntmat.t[:], in0=jf, scalar1=iop.t[:, 0:1], scalar2=-1.0, op0=ALU.is_lt, op1=ALU.mult), cb, ntmat.b)
        P.op("dve", lambda e: e.tensor_scalar(out=umat.t[:], in0=jf, scalar1=iop.t[:, 0:1], scalar2=None, op0=ALU.is_gt), cb, umat.b)
        P.op("dve", lambda e: e.tensor_copy(out=iosl.t[:], in_=jf), cb, iosl.b)
        P.op("pool", lambda e: e.memset(bd_f.t[:], 0.0), writes=bd_f.b)
        P.op("pool", lambda e: e.memset(bd_f.t[0:64, 0:64], 1.0), bd_f.b, bd_f.b)
        P.op("pool", lambda e: e.memset(bd_f.t[64:128, 64:128], 1.0), bd_f.b, bd_f.b)

        P.barrier()
        cst.close()
        xv = D["x"].rearrange("(tt p) d -> p tt d", p=128)
        for q4 in range(4):
            P.dma("sp", lambda e, q4=q4: e.dma_start(out=X.t[:, q4 * 4:(q4 + 1) * 4, :], in_=xv[:, q4 * 4:(q4 + 1) * 4, :]),
                  writes=X.b[q4 * 4:(q4 + 1) * 4])

        def bcast_load(dst, src_row, n):
            P.dma("sp", lambda e: e.dma_start(out=dst.t[:, 0:n], in_=src_row.partition_broadcast(128)), writes=dst.b)

        def wload(dst, src, kchunks, c0, c1):
            v = src.rearrange("(k p) n -> p k n", p=128)
            step = max(1, 2048 // (c1 - c0))
            for k0 in range(0, kchunks, step):
                k1 = min(kchunks, k0 + step)
                P.dma("pool", lambda e, k0=k0, k1=k1: e.dma_start(out=dst.t[:, k0:k1, :], in_=v[:, k0:k1, c0:c1]),
                      writes=dst.b)

        def build_xT(xT, extra=None):
            for tt in range(16):
                for hf in range(2):
                    pt = ps[hf]
                    for j in range(4):
                        k = hf * 4 + j
                        P.op("pe", lambda e, pt=pt, j=j, k=k, tt=tt: e.transpose(out=pt.t[:, j * 128:(j + 1) * 128], in_=X.t[:, tt, k * 128:(k + 1) * 128], identity=ident.t[:]),
                             [X.b[tt]] + ident.b, pt.b)
                    copy(cp_engine(), xT.t[:, hf * 4:(hf + 1) * 4, tt * 128:(tt + 1) * 128],
                         pt.t[:].rearrange("p (j t) -> p j t", j=4), pt.b, xT.b)
                    if extra is not None:
                        extra(tt, hf, pt)

        def rsqrt_(ap, bufs):
            P.op("act", lambda e: e.activation(out=ap, in_=ap, func=AF.Ln), bufs, bufs)
            P.op("act", lambda e: e.activation(out=ap, in_=ap, func=AF.Exp, scale=-0.5), bufs, bufs)

        def ln_tile(st_, r, rb, tt, gbc, bbc, tmp):
            stt, mv, rs, nmr, xn = tmp
            P.op("dve", lambda e: e.bn_stats(out=stt.t[:, 0, :], in_=r[:, 0:512]), rb, stt.b)
            P.op("dve", lambda e: e.bn_stats(out=stt.t[:, 1, :], in_=r[:, 512:1024]), rb, stt.b)
            P.op("dve", lambda e: e.bn_aggr(out=mv.t[:], in_=stt.t[:].rearrange("p a b -> p (a b)")), stt.b, mv.b)
            P.op("dve", lambda e: e.tensor_scalar(out=rs.t[:], in0=mv.t[:, 1:2], scalar1=LN_EPS, scalar2=None, op0=ALU.add), mv.b, rs.b)
            rsqrt_(rs.t[:], rs.b)
            P.op("dve", lambda e: e.scalar_tensor_tensor(out=nmr.t[:], in0=mv.t[:, 0:1], scalar=-1.0, in1=rs.t[:], op0=ALU.mult, op1=ALU.mult), mv.b + rs.b, nmr.b)
            P.op("act", lambda e: e.activation(out=xn.t[:], in_=r, func=AF.Identity, bias=nmr.t[:, 0:1], scale=rs.t[:, 0:1]), rb + nmr.b + rs.b, xn.b)
            P.op("pool", lambda e: e.tensor_tensor(out=xn.t[:], in0=xn.t[:], in1=gbc.t[:], op=ALU.mult), xn.b + gbc.b, xn.b)
            P.op("dve", lambda e: e.tensor_tensor(out=X.t[:, tt, :], in0=xn.t[:], in1=bbc.t[:], op=ALU.add), xn.b + bbc.b, [X.b[tt]])

        def ln_tmps(st_, tag):
            return [(sb(st_, "lnst%s%d" % (tag, i), [128, 2, 6]), sb(st_, "lnmv%s%d" % (tag, i), [128, 2]),
                     sb(st_, "lnrs%s%d" % (tag, i), [128, 1]), sb(st_, "lnnm%s%d" % (tag, i), [128, 1]),
                     sb(st_, "lnxn%s%d" % (tag, i), [128, 1024])) for i in range(2)]

        def proj_ln(st_, srcT, w, l, li, tag):
            gbc = sb(st_, "gbc" + tag, [128, 1024]); bbc = sb(st_, "bbc" + tag, [128, 1024])
            bcast_load(gbc, D["ln_g"][l, li:li + 1, :], 1024)
            bcast_load(bbc, D["ln_b"][l, li:li + 1, :], 1024)
            tmps = ln_tmps(st_, tag)
            rbuf = [sb(st_, "r%s%d" % (tag, i), [128, 1024]) for i in range(2)]
            for tt in range(16):
                r = rbuf[tt % 2]
                for dh in range(2):
                    pm = ps[2 + (tt * 2 + dh) % 4]
                    for c in range(8):
                        P.op("pe", lambda e, pm=pm, c=c, tt=tt, dh=dh: e.matmul(pm.t[:], srcT.t[:, c, tt * 128:(tt + 1) * 128], w.t[:, c, dh * 512:(dh + 1) * 512], start=(c == 0), stop=(c == 7)),
                             srcT.b + w.b, pm.b)
                    P.op("dve", lambda e, pm=pm, r=r, tt=tt, dh=dh: e.scalar_tensor_tensor(out=r.t[:, dh * 512:(dh + 1) * 512], in0=X.t[:, tt, dh * 512:(dh + 1) * 512], scalar=ALPHA, in1=pm.t[:], op0=ALU.mult, op1=ALU.add),
                         [X.b[tt]] + pm.b, r.b)
                ln_tile(st_, r.t[:], r.b, tt, gbc, bbc, tmps[tt % 2])

        for l in range(n_layers):
            with contextlib.ExitStack() as mx:
                yT = sb(mx, "yT", [128, 8, 2048], BF16)
                grpc = sb(mx, "grpc", [128, 8])
                P.dma("sp", lambda e: e.dma_start(out=grpc.t[:], in_=D["grp_g"][l].rearrange("(c p) -> p c", p=128)), writes=grpc.b)
                with contextlib.ExitStack() as ab:
                    qT = sb(ab, "qT", [128, 4, 2048], BF16)
                    kT = sb(ab, "kT", [128, 4, 2048], BF16)
                    V = sb(ab, "V", [128, 16, 512], BF16)
                    with contextlib.ExitStack() as a1:
                        xT = sb(a1, "xT", [128, 8, 2048], BF16)
                        wq3 = [sb(a1, "wqkv%d" % i, [128, 8, 512], BF16) for i in range(2)]
                        wload(wq3[0], D["w_in"][l], 8, 1024, 1536)
                        wload(wq3[1], D["w_in"][l], 8, 1536, 2048)
                        build_xT(xT)
                        for c in range(8):
                            wp = wq3[c // 4]
                            for tb in range(4):
                                pm = ps[2 + (c * 4 + tb) % 4]
                                for k in range(8):
                                    P.op("pe", lambda e, pm=pm, c=c, tb=tb, k=k, wp=wp: e.matmul(pm.t[:], wp.t[:, k, (c % 4) * 128:(c % 4 + 1) * 128], xT.t[:, k, tb * 512:(tb + 1) * 512], start=(k == 0), stop=(k == 7)),
                                         wp.b + xT.b, pm.b)
                                if c < 4:
                                    copy(cp_engine(), qT.t[:, c, tb * 512:(tb + 1) * 512], pm.t[:], pm.b, qT.b, scale=0.125)
                                else:
                                    copy(cp_engine(), kT.t[:, c - 4, tb * 512:(tb + 1) * 512], pm.t[:], pm.b, kT.b)
                            if c == 3:
                                wload(wq3[0], D["w_in"][l], 8, 2048, 2560)
                        for tt in range(16):
                            pm = ps[2 + tt % 4]
                            for k in range(8):
                                P.op("pe", lambda e, pm=pm, tt=tt, k=k: e.matmul(pm.t[:], xT.t[:, k, tt * 128:(tt + 1) * 128], wq3[0].t[:, k, :], start=(k == 0), stop=(k == 7)),
                                     wq3[0].b + xT.b, pm.b)
                            copy(cp_engine(), V.t[:, tt, :], pm.t[:], pm.b, V.b)
                        P.barrier()
                    if stop == "a1q":
                        P.op("dve", lambda e: e.tensor_copy(out=X.t[:, 0:8, :].rearrange("p a b -> p (a b)"), in_=qT.t[:].rearrange("p a b -> p (a b)")), qT.b + X.b[0:8], X.b[0:8])
                        P.op("dve", lambda e: e.tensor_copy(out=X.t[:, 8:16, :].rearrange("p a b -> p (a b)"), in_=kT.t[:].rearrange("p a b -> p (a b)")), kT.b + X.b[8:16], X.b[8:16])
                        P.barrier()
                    with contextlib.ExitStack() as bb:
                      if stop not in ("a1", "a1q"):
                        NB = 2
                        Esb = [sb(bb, "Esb%d" % i, [128, 512]) for i in range(NB)]
                        Lb = [sb(bb, "Lb%d" % i, [128, 512], BF16) for i in range(4)]
                        tmp = [sb(bb, "tmpb%d" % i, [128, 512]) for i in range(NB)]
                        aT = [sb(bb, "aT%d" % i, [128, 512], BF16) for i in range(NB)]
                        accb = [sb(bb, "accb%d" % i, [128, 2048], BF16, nb=4) for i in range(1)] * 2
                        outT = [sb(bb, "outT%d" % i, [128, 512]) for i in range(2)]
                        sq = [sb(bb, "sq%d" % i, [128, 512]) for i in range(1)] * 2
                        rsd = [sb(bb, "rsd%d" % i, [128, 512]) for i in range(1)] * 2
                        kz = [sb(bb, "kz%d" % i, [128, 2048], BF16) for i in range(2)]
                        Vz = [sb(bb, "Vz%d" % i, [128, 16, 128], BF16) for i in range(2)]
                        for i in range(2):
                            P.op("pool", lambda e, i=i: e.memset(kz[i].t[:], 0.0), writes=kz[i].b)
                            P.op("pool", lambda e, i=i: e.memset(Vz[i].t[:], 0.0), writes=Vz[i].b)
                        un = 0
                        import os
                        BK = int(os.environ.get('BK', '0')); BHP = int(os.environ.get('BHP', '4')); BEPI = int(os.environ.get('BEPI', '1')); BC = int(os.environ.get('BC', '0')); BKMAX = int(os.environ.get('BKMAX', '15'))
                        qA = []
                        qB = []
                        qC = []

                        def drainA():
                            cv, f1, fb, fc = qA.pop(0)
                            f1()
                            qB.append((cv, fb, fc))

                        def drainB():
                            cv, fb, fc = qB.pop(0)
                            while any(pc == cv for pc, _ in qC):
                                qC.pop(0)[1]()
                            fb()
                            qC.append((cv, fc))

                        def make_unit(hp, hi, kb, c, un, ac):
                            diag = (c == kb // 4)
                            r_ = kb % 4
                            o0 = 384 - 128 * r_
                            first = (kb == 4 * c + 3)
                            zp = ps[4 + un % 4]
                            op_ = ps[c]
                            E_, L_, t_, a_ = Esb[un % NB], Lb[un % 4], tmp[un % NB], aT[un % NB]

                            def s0():
                                P.op("pe", lambda e: e.matmul(zp.t[:], kz[hi].t[:, kb * 128:(kb + 1) * 128], qT.t[:, hp, c * 512:(c + 1) * 512], start=True, stop=True),
                                     kz[hi].b + qT.b, zp.b)

                            def s1():
                                P.op("act", lambda e: e.activation(out=E_.t[:], in_=zp.t[:], func=AF.Exp), zp.b, E_.b)
                                P.op("act", lambda e: e.activation(out=L_.t[:], in_=E_.t[:], func=AF.Ln, bias=1.0), E_.b, L_.b)
                                if diag:
                                    P.op("pool", lambda e: e.tensor_tensor(out=L_.t[:], in0=L_.t[:], in1=m01.t[:, o0:o0 + 512], op=ALU.mult), L_.b + m01.b, L_.b)

                            def s1b():
                                P.op("pe", lambda e: e.matmul(zp.t[:], ntmat.t[:], L_.t[:], start=False, stop=first, skip_group_check=True), L_.b + ntmat.b, zp.b)
                                if not first:
                                    P.op("pe", lambda e: e.matmul(zp.t[:], nones_b.t[:], ac.t[:, c * 512:(c + 1) * 512], start=False, stop=True, skip_group_check=True), [ac.b[c]] + nones_b.b, zp.b)

                            def s2():
                                P.op("dve", lambda e: e.tensor_tensor(out=t_.t[:], in0=zp.t[:], in1=L_.t[:], op=ALU.subtract), zp.b + L_.b, t_.b)
                                if diag:
                                    P.op("dve", lambda e: e.tensor_tensor(out=t_.t[:], in0=t_.t[:], in1=mneg.t[:, o0:o0 + 512], op=ALU.add), t_.b + mneg.b, t_.b)
                                P.op("act", lambda e: e.activation(out=a_.t[:], in_=t_.t[:], func=AF.Exp), t_.b, a_.b)
                                P.op("pe", lambda e: e.matmul(op_.t[:], Vz[hi].t[:, kb, :], a_.t[:], start=(first and hi == 0), stop=(kb == 0 and hi == 1)),
                                     Vz[hi].b + a_.b, op_.b)
                                if kb > 0:
                                    if first:
                                        P.op("pool", lambda e: e.tensor_copy(out=ac.t[:, c * 512:(c + 1) * 512], in_=L_.t[:]), L_.b, [ac.b[c]])
                                    else:
                                        P.op("pool", lambda e: e.tensor_tensor(out=ac.t[:, c * 512:(c + 1) * 512], in0=ac.t[:, c * 512:(c + 1) * 512], in1=L_.t[:], op=ALU.add), L_.b + [ac.b[c]], [ac.b[c]])
                            return s0, s1, s1b, s2

                        for hp in range(BHP):
                            for hi in range(2):
                                h = hp * 2 + hi
                                hs = hi * 64
                                ac = accb[hi]
                                P.op("pool", lambda e, hi=hi, hs=hs, hp=hp: e.tensor_copy(out=kz[hi].t[hs:hs + 64, :], in_=kT.t[hs:hs + 64, hp, :]), kT.b + kz[hi].b, kz[hi].b)
                                P.op("pool", lambda e, hi=hi, hs=hs, h=h: e.tensor_copy(out=Vz[hi].t[:, :, hs:hs + 64], in_=V.t[:, :, h * 64:(h + 1) * 64]), V.b + Vz[hi].b, Vz[hi].b)
                                for kb in range(BKMAX, BK - 1, -1):
                                    for c in range(3, max(BC, kb // 4) - 1, -1):
                                        s0, s1, s1b, s2 = make_unit(hp, hi, kb, c, un, ac)
                                        un += 1
                                        s0()
                                        qA.append((c, s1, s1b, s2))
                                        while len(qA) > 1:
                                            drainA()
                                        while len(qB) > 1:
                                            drainB()
                                        while len(qC) > 1:
                                            qC.pop(0)[1]()
                            while qA:
                                drainA()
                            while qB:
                                drainB()
                            while qC:
                                qC.pop(0)[1]()
                            if os.environ.get('BDUMP') == '1' and hp == 0:
                                for c in range(4):
                                    P.op("dve", lambda e, c=c: e.tensor_copy(out=X.t[:, c, 0:512], in_=ps[c].t[:]), ps[c].b + [X.b[c]], [X.b[c]])
                            EPS = int(os.environ.get('EPS', '9')); EPC0 = int(os.environ.get('EPC0', '0'))
                            for c in range(EPC0, 4 if BEPI else 0):
                                op_ = ps[c]
                                sq_, rs_ = sq[c % 2], rsd[c % 2]
                                sp_ = ps[4 + c % 2]
                                P.op("dve", lambda e, op_=op_, c=c: e.tensor_copy(out=outT[c % 2].t[:], in_=op_.t[:]), op_.b, outT[c % 2].b)
                                if EPS >= 1:
                                    P.op("dve", lambda e, c=c, sq_=sq_: e.tensor_tensor(out=sq_.t[:], in0=outT[c % 2].t[:], in1=outT[c % 2].t[:], op=ALU.mult), outT[c % 2].b, sq_.b)
                                if EPS >= 2:
                                    P.op("pe", lambda e, sp_=sp_, sq_=sq_: e.matmul(sp_.t[:], bd_f.t[:], sq_.t[:], start=True, stop=True), sq_.b + bd_f.b, sp_.b)
                                if EPS >= 3:
                                    P.op("dve", lambda e, sp_=sp_, rs_=rs_: e.tensor_scalar(out=rs_.t[:], in0=sp_.t[:], scalar1=1.0 / 64.0, scalar2=RMS_EPS, op0=ALU.mult, op1=ALU.add), sp_.b, rs_.b)
                                if EPS >= 4:
                                    rsqrt_(rs_.t[:], rs_.b)
                                if os.environ.get('BDUMP') == '2' and hp == 0:
                                    P.op("dve", lambda e, c=c: e.tensor_copy(out=X.t[:, 4 + c, 0:512], in_=outT[c % 2].t[:]), outT[c % 2].b + [X.b[4 + c]], [X.b[4 + c]])
                                    P.op("dve", lambda e, c=c, rs_=rs_: e.tensor_copy(out=X.t[:, 8 + c, 0:512], in_=rs_.t[:]), rs_.b + [X.b[8 + c]], [X.b[8 + c]])
                                    P.op("dve", lambda e, c=c, sq_=sq_: e.tensor_copy(out=X.t[:, 12 + c, 0:512], in_=sq_.t[:]), sq_.b + [X.b[12 + c]], [X.b[12 + c]])
                                    P.op("dve", lambda e, c=c: e.tensor_copy(out=X.t[:, 12 + c, 512:520], in_=grpc.t[:]), grpc.b + [X.b[12 + c]], [X.b[12 + c]])
                                if os.environ.get('BDUMP') == '3' and hp == 0:
                                    P.op("dve", lambda e, rs_=rs_, c=c, hp=hp: e.scalar_tensor_tensor(out=yT.t[:, 4 + hp, c * 512:(c + 1) * 512], in0=outT[c % 2].t[:], scalar=grpc.t[:, 4 + hp:5 + hp], in1=rs_.t[:], op0=ALU.mult, op1=ALU.mult),
                                         outT[c % 2].b + rs_.b + grpc.b, yT.b)
                                    P.op("dve", lambda e, c=c: e.tensor_copy(out=X.t[:, 4 + c, 0:512], in_=outT[c % 2].t[:]), outT[c % 2].b + [X.b[4 + c]], [X.b[4 + c]])
                                    P.op("dve", lambda e, c=c, rs_=rs_: e.tensor_copy(out=X.t[:, 8 + c, 0:512], in_=rs_.t[:]), rs_.b + [X.b[8 + c]], [X.b[8 + c]])
                                    P.op("dve", lambda e, c=c, hp=hp: e.tensor_copy(out=X.t[:, 12 + c, 0:512], in_=yT.t[:, 4 + hp, c * 512:(c + 1) * 512]), yT.b + [X.b[12 + c]], [X.b[12 + c]])
                                    P.op("dve", lambda e, c=c: e.tensor_copy(out=X.t[:, 12 + c, 512:520], in_=grpc.t[:]), grpc.b + [X.b[12 + c]], [X.b[12 + c]])
                                    continue
                                if EPS >= 5:
                                    P.op("dve", lambda e, rs_=rs_, c=c, hp=hp: e.scalar_tensor_tensor(out=yT.t[:, 4 + hp, c * 512:(c + 1) * 512], in0=outT[c % 2].t[:], scalar=grpc.t[:, 4 + hp:5 + hp], in1=rs_.t[:], op0=ALU.mult, op1=ALU.mult),
                                         outT[c % 2].b + rs_.b + grpc.b, yT.b)
                        P.barrier()
                with contextlib.ExitStack() as a2:
                  if stop not in ("a1", "a1q", "b", "by"):
                    xT = sb(a2, "xT2", [128, 8, 2048], BF16)
                    wuv = sb(a2, "wuv", [128, 8, 1024], BF16)
                    wload(wuv, D["w_in"][l], 8, 0, 1024)
                    wsp = sb(a2, "wsp", [128, 4, 128])
                    wcT = sb(a2, "wcT", [128, 4, 128], BF16)
                    bsp = sb(a2, "bsp", [128, 4])
                    sg_bc = sb(a2, "sg_bc", [128, 512]); sb_bc = sb(a2, "sb_bc", [128, 512]); gg_bc = sb(a2, "gg_bc", [128, 512])
                    bcast_load(sg_bc, D["sgu_g"][l:l + 1, :], 512)
                    bcast_load(sb_bc, D["sgu_b"][l:l + 1, :], 512)
                    bcast_load(gg_bc, D["grp_g"][l:l + 1, 0:512], 512)
                    P.dma("sp", lambda e: e.dma_start(out=wsp.t[:], in_=D["w_sp"][l].rearrange("g t s -> t g s")), writes=wsp.b)
                    P.dma("sp", lambda e: e.dma_start(out=bsp.t[:], in_=D["b_sp"][l].rearrange("g t -> t g")), writes=bsp.b)
                    for g in range(4):
                        P.op("pe", lambda e, g=g: e.transpose(out=ps[0].t[:, g * 128:(g + 1) * 128], in_=wsp.t[:, g, :], identity=ident.t[:]), wsp.b + ident.b, ps[0].b)
                    for g in range(4):
                        P.op("dve", lambda e, g=g: e.tensor_tensor(out=wcT.t[:, g, :], in0=ps[0].t[:, g * 128:(g + 1) * 128], in1=cmask.t[:], op=ALU.mult), ps[0].b + cmask.b, wcT.b)
                    build_xT(xT)
                    ug = [sb(a2, "ug%d" % i, [128, 512]) for i in range(2)]
                    vg = [sb(a2, "vg%d" % i, [128, 512]) for i in range(2)]
                    vnb = [sb(a2, "vnb%d" % i, [128, 512], BF16) for i in range(2)]
                    oa = [sb(a2, "oa%d" % i, [128, 512]) for i in range(2)]
                    junk = [sb(a2, "junk%d" % i, [128, 128]) for i in range(2)]
                    st4 = [sb(a2, "st4%d" % i, [128, 4, 6]) for i in range(2)]
                    mv4 = [sb(a2, "mv4%d" % i, [128, 4, 2]) for i in range(2)]
                    rs4 = [sb(a2, "rs4%d" % i, [128, 4]) for i in range(2)]
                    ssq = [sb(a2, "ssq%d" % i, [128, 4]) for i in range(2)]
                    import os
                    for tt in range(int(os.environ.get('A2T', '16'))):
                        i2 = tt % 2
                        pu, pv, pg, pt = ps[2 + i2 * 2], ps[3 + i2 * 2], ps[6], ps[7]
                        u_, v_, n_, o_, s_, m_, r_, q_ = ug[i2], vg[i2], vnb[i2], oa[i2], st4[i2], mv4[i2], rs4[i2], ssq[i2]
                        for k in range(8):
                            P.op("pe", lambda e, pu=pu, k=k, tt=tt: e.matmul(pu.t[:], xT.t[:, k, tt * 128:(tt + 1) * 128], wuv.t[:, k, 0:512], start=(k == 0), stop=(k == 7)), xT.b + wuv.b, pu.b)
                        for k in range(8):
                            P.op("pe", lambda e, pv=pv, k=k, tt=tt: e.matmul(pv.t[:], xT.t[:, k, tt * 128:(tt + 1) * 128], wuv.t[:, k, 512:1024], start=(k == 0), stop=(k == 7)), xT.b + wuv.b, pv.b)
                        P.op("act", lambda e, pu=pu, u_=u_: e.activation(out=u_.t[:], in_=pu.t[:], func=AF.Gelu_apprx_tanh), pu.b, u_.b)
                        P.op("act", lambda e, pv=pv, v_=v_: e.activation(out=v_.t[:], in_=pv.t[:], func=AF.Gelu_apprx_tanh), pv.b, v_.b)
                        for g in range(4):
                            P.op("dve", lambda e, g=g, s_=s_, v_=v_: e.bn_stats(out=s_.t[:, g, :], in_=v_.t[:, g * 128:(g + 1) * 128]), v_.b, s_.b)
                        for g in range(4):
                            P.op("dve", lambda e, g=g, s_=s_, m_=m_: e.bn_aggr(out=m_.t[:, g, :], in_=s_.t[:, g, :]), s_.b, m_.b)
                        P.op("dve", lambda e, m_=m_, r_=r_: e.tensor_scalar(out=r_.t[:], in0=m_.t[:, :, 1], scalar1=LN_EPS, scalar2=None, op0=ALU.add), m_.b, r_.b)
                        rsqrt_(r_.t[:], r_.b)
                        for g in range(4):
                            P.op("dve", lambda e, g=g, v_=v_, m_=m_, r_=r_: e.tensor_scalar(out=v_.t[:, g * 128:(g + 1) * 128], in0=v_.t[:, g * 128:(g + 1) * 128], scalar1=m_.t[:, g, 0:1], scalar2=r_.t[:, g:g + 1], op0=ALU.subtract, op1=ALU.mult),
                                 v_.b + m_.b + r_.b, v_.b)
                        P.op("pool", lambda e, v_=v_: e.tensor_tensor(out=v_.t[:], in0=v_.t[:], in1=sg_bc.t[:], op=ALU.mult), v_.b + sg_bc.b, v_.b)
                        P.op("dve", lambda e, v_=v_, n_=n_: e.tensor_tensor(out=n_.t[:], in0=v_.t[:], in1=sb_bc.t[:], op=ALU.add), v_.b + sb_bc.b, n_.b)
                        for g in range(4):
                            P.op("pe", lambda e, g=g, pg=pg, n_=n_: e.matmul(pg.t[:, g * 128:(g + 1) * 128], wcT.t[:, g, :], n_.t[:, g * 128:(g + 1) * 128], start=True, stop=True), wcT.b + n_.b, pg.b)
                        for g in range(4):
                            P.op("dve", lambda e, g=g, pg=pg, o_=o_, u_=u_: e.scalar_tensor_tensor(out=o_.t[:, g * 128:(g + 1) * 128], in0=pg.t[:, g * 128:(g + 1) * 128], scalar=bsp.t[:, g:g + 1], in1=u_.t[:, g * 128:(g + 1) * 128], op0=ALU.add, op1=ALU.mult),
                                 pg.b + bsp.b + u_.b, o_.b)
                        for g in range(4):
                            P.op("dve", lambda e, g=g, o_=o_, q_=q_, i2=i2: e.scalar_tensor_tensor(out=junk[i2].t[:], in0=o_.t[:, g * 128:(g + 1) * 128], scalar=1.0, in1=o_.t[:, g * 128:(g + 1) * 128], op0=ALU.mult, op1=ALU.mult, accum_out=q_.t[:, g:g + 1]), o_.b, q_.b + junk[i2].b)
                        P.op("dve", lambda e, q_=q_: e.tensor_scalar(out=q_.t[:], in0=q_.t[:], scalar1=1.0 / 128.0, scalar2=RMS_EPS, op0=ALU.mult, op1=ALU.add), q_.b, q_.b)
                        rsqrt_(q_.t[:], q_.b)
                        for g in range(4):
                            P.op("dve", lambda e, g=g, o_=o_, q_=q_: e.scalar_tensor_tensor(out=o_.t[:, g * 128:(g + 1) * 128], in0=o_.t[:, g * 128:(g + 1) * 128], scalar=q_.t[:, g:g + 1], in1=gg_bc.t[:, g * 128:(g + 1) * 128], op0=ALU.mult, op1=ALU.mult),
                                 o_.b + q_.b + gg_bc.b, o_.b)
                        for g in range(4):
                            P.op("pe", lambda e, g=g, pt=pt, o_=o_: e.transpose(out=pt.t[:, g * 128:(g + 1) * 128], in_=o_.t[:, g * 128:(g + 1) * 128], identity=ident.t[:]), o_.b + ident.b, pt.b)
                        copy("act", yT.t[:, 0:4, tt * 128:(tt + 1) * 128], pt.t[:].rearrange("p (j t) -> p j t", j=4), pt.b, yT.b)
                    P.barrier()
                if stop in ("a2y", "by"):
                    for c8 in range(8):
                        P.op("dve", lambda e, c8=c8: e.tensor_copy(out=X.t[:, 2 * c8:2 * c8 + 2, :].rearrange("p a b -> p (a b)"), in_=yT.t[:, c8, :]), yT.b + X.b, X.b)
                    P.barrier()
                with contextlib.ExitStack() as cc:
                  if stop not in ("a1", "a1q", "b", "a2", "a2y", "by"):
                    wo_ = sb(cc, "wout", [128, 8, 1024], BF16)
                    wload(wo_, D["w_out"][l], 8, 0, 1024)
                    proj_ln(cc, yT, wo_, l, 0, "c")
                    P.barrier()
            if stop in ("mixer%d" % l, "a1", "a1q", "b", "a2", "a2y", "by"):
                break
            with contextlib.ExitStack() as md:
                kTm = sb(md, "kTm", [128, 8, 256], BF16)
                Vm = sb(md, "Vm", [128, 2, 1024], BF16)
                qmT = sb(md, "qmT", [128, 8, 2048], BF16)
                with contextlib.ExitStack() as d1:
                    wkv = sb(d1, "wkv", [128, 8, 2048], BF16)
                    wload(wkv, D["wkv_mem"][l], 8, 0, 2048)
                    memf = sb(d1, "memf", [128, 2, 1024])
                    memT = sb(d1, "memT", [128, 8, 256], BF16)
                    P.dma("sp", lambda e: e.dma_start(out=memf.t[:], in_=D["mem"].rearrange("(mt p) d -> p mt d", p=128)), writes=memf.b)
                    for mt in range(2):
                        for hf in range(2):
                            pt = ps[hf]
                            for j in range(4):
                                k = hf * 4 + j
                                P.op("pe", lambda e, pt=pt, j=j, k=k, mt=mt: e.transpose(out=pt.t[:, j * 128:(j + 1) * 128], in_=memf.t[:, mt, k * 128:(k + 1) * 128], identity=ident.t[:]), memf.b + ident.b, pt.b)
                            copy(cp_engine(), memT.t[:, hf * 4:(hf + 1) * 4, mt * 128:(mt + 1) * 128], pt.t[:].rearrange("p (j t) -> p j t", j=4), pt.b, memT.b)
                    for c in range(8):
                        pm = ps[2 + c % 4]
                        for k in range(8):
                            P.op("pe", lambda e, pm=pm, c=c, k=k: e.matmul(pm.t[:, 0:256], wkv.t[:, k, c * 128:(c + 1) * 128], memT.t[:, k, :], start=(k == 0), stop=(k == 7)), wkv.b + memT.b, pm.b)
                        copy(cp_engine(), kTm.t[:, c, :], pm.t[:, 0:256], pm.b, kTm.b)
                    for mt in range(2):
                        for dh in range(2):
                            pm = ps[2 + (mt * 2 + dh) % 4]
                            for k in range(8):
                                P.op("pe", lambda e, pm=pm, mt=mt, dh=dh, k=k: e.matmul(pm.t[:], memT.t[:, k, mt * 128:(mt + 1) * 128], wkv.t[:, k, 1024 + dh * 512:1024 + (dh + 1) * 512], start=(k == 0), stop=(k == 7)), wkv.b + memT.b, pm.b)
                            copy(cp_engine(), Vm.t[:, mt, dh * 512:(dh + 1) * 512], pm.t[:], pm.b, Vm.b)
                    P.barrier()
                with contextlib.ExitStack() as d2:
                    xT = sb(d2, "xT3", [128, 8, 2048], BF16)
                    wq = sb(d2, "wq", [128, 8, 1024], BF16)
                    wload(wq, D["wq_mem"][l], 8, 0, 1024)
                    build_xT(xT)
                    for c in range(8):
                        for tb in range(4):
                            pm = ps[2 + (c * 4 + tb) % 4]
                            for k in range(8):
                                P.op("pe", lambda e, pm=pm, c=c, tb=tb, k=k: e.matmul(pm.t[:], wq.t[:, k, c * 128:(c + 1) * 128], xT.t[:, k, tb * 512:(tb + 1) * 512], start=(k == 0), stop=(k == 7)), wq.b + xT.b, pm.b)
                            copy(cp_engine(), qmT.t[:, c, tb * 512:(tb + 1) * 512], pm.t[:], pm.b, qmT.b)
                    P.barrier()
                with contextlib.ExitStack() as d3:
                    OT = sb(d3, "OT", [128, 8, 2048], BF16)
                    wo_ = sb(d3, "womem", [128, 8, 1024], BF16)
                    wload(wo_, D["wo_mem"][l], 8, 0, 1024)
                    PT = [sb(d3, "PT%d" % i, [128, 2, 512], BF16) for i in range(2)]
                    rinv = [sb(d3, "rinv%d" % i, [128, 512]) for i in range(2)]
                    un = 0
                    for h in range(4):
                        for tb in range(4):
                            p_, ri_ = PT[un % 2], rinv[un % 2]
                            un += 1
                            for mt in range(2):
                                sp_ = ps[mt]
                                for c2 in range(2):
                                    P.op("pe", lambda e, sp_=sp_, mt=mt, c2=c2, h=h, tb=tb: e.matmul(sp_.t[:], kTm.t[:, 2 * h + c2, mt * 128:(mt + 1) * 128], qmT.t[:, 2 * h + c2, tb * 512:(tb + 1) * 512], start=(c2 == 0), stop=(c2 == 1)), kTm.b + qmT.b, sp_.b)
                                P.op("act", lambda e, sp_=sp_, p_=p_, mt=mt: e.activation(out=p_.t[:, mt, :], in_=sp_.t[:], func=AF.Exp, scale=1.0 / 16.0), sp_.b, p_.b)
                            sm = ps[2 + un % 2]
                            for mt in range(2):
                                P.op("pe", lambda e, sm=sm, p_=p_, mt=mt: e.matmul(sm.t[:], ones_b.t[:], p_.t[:, mt, :], start=(mt == 0), stop=(mt == 1)), p_.b + ones_b.b, sm.b)
                            P.op("dve", lambda e, sm=sm, ri_=ri_: e.reciprocal(out=ri_.t[:], in_=sm.t[:]), sm.b, ri_.b)
                            for c2 in range(2):
                                po = ps[4 + (un * 2 + c2) % 4]
                                for mt in range(2):
                                    P.op("pe", lambda e, po=po, p_=p_, mt=mt, h=h, c2=c2: e.matmul(po.t[:], Vm.t[:, mt, h * 256 + c2 * 128:h * 256 + (c2 + 1) * 128], p_.t[:, mt, :], start=(mt == 0), stop=(mt == 1)), p_.b + Vm.b, po.b)
                                P.op("dve", lambda e, po=po, ri_=ri_, h=h, c2=c2, tb=tb: e.tensor_tensor(out=OT.t[:, 2 * h + c2, tb * 512:(tb + 1) * 512], in0=po.t[:], in1=ri_.t[:], op=ALU.mult), po.b + ri_.b, OT.b)
                    proj_ln(d3, OT, wo_, l, 1, "d")
                    P.barrier()
            if stop == "mem%d" % l:
                break
            with contextlib.ExitStack() as me:
                Xb = sb(me, "Xb", [128, 16, 1024], BF16)
                POS = sb(me, "POS", [128, 16, 32])
                Ga = sb(me, "Ga", [128, 16, 32])
                bgu = sb(me, "bgu", [128, 16, 32])
                with contextlib.ExitStack() as e0:
                    wr = sb(e0, "wr", [128, 8, 32])
                    P.dma("sp", lambda e: e.dma_start(out=wr.t[:], in_=D["w_router"][l].rearrange("(k p) n -> p k n", p=128)), writes=wr.b)
                    wrh = sb(e0, "wrh", [128, 8, 32], BF16)
                    wrl = sb(e0, "wrl", [128, 8, 32], BF16)
                    P.op("dve", lambda e: e.tensor_copy(out=wrh.t[:], in_=wr.t[:]), wr.b, wrh.b)
                    P.op("dve", lambda e: e.tensor_tensor(out=wrl.t[:], in0=wr.t[:], in1=wrh.t[:], op=ALU.subtract), wr.b + wrh.b, wrl.b)
                    br_bc = sb(e0, "br_bc", [128, 32])
                    bcast_load(br_bc, D["b_router"][l:l + 1, :], 32)
                    bgun = sb(e0, "bgun", [128, 2048])
                    P.op("pool", lambda e: e.memset(bgun.t[:], 0.0), writes=bgun.b)
                    P.dma("sp", lambda e: e.dma_start(out=bgun.t[0:32, :], in_=D["b_gu"][l]), writes=bgun.b)
                    bdn = sb(e0, "bdn", [128, 1024])
                    P.op("pool", lambda e: e.memset(bdn.t[:], 0.0), writes=bdn.b)
                    P.dma("sp", lambda e: e.dma_start(out=bdn.t[0:32, :], in_=D["b_down"][l]), writes=bdn.b)
                    bdnb = sb(e0, "bdnb", [128, 1024], BF16)
                    P.op("dve", lambda e: e.tensor_copy(out=bdnb.t[:], in_=bdn.t[:]), bdn.b, bdnb.b)
                    for c in range(16):
                        pq = ps[4 + c % 4]
                        P.op("pe", lambda e, c=c, pq=pq: e.transpose(out=pq.t[:, 0:128], in_=bgun.t[:, c * 128:(c + 1) * 128], identity=ident.t[:]), bgun.b + ident.b, pq.b)
                        P.op("dve", lambda e, c=c, pq=pq: e.tensor_copy(out=bgu.t[:, c, :], in_=pq.t[:, 0:32]), pq.b, bgu.b)
                    Gp = [sb(e0, "Gp%d" % i, [128, 128]) for i in range(2)]
                    for i in range(2):
                        P.op("pool", lambda e, i=i: e.memset(Gp[i].t[:], 0.0), writes=Gp[i].b)
                    maskb = sb(e0, "maskb", [128, 16, 32], BF16, nb=16)
                    xTt = [sb(e0, "xTt%d" % i, [128, 8, 128], BF16) for i in range(2)]
                    xTf = [sb(e0, "xTf%d" % i, [128, 8, 128], BF16) for i in range(2)]
                    lg = [sb(e0, "lg%d" % i, [128, 32]) for i in range(2)]
                    t8 = [sb(e0, "t8%d" % i, [128, 8]) for i in range(2)]
                    mk = [sb(e0, "mk%d" % i, [128, 32]) for i in range(2)]
                    ex = [sb(e0, "ex%d" % i, [128, 32]) for i in range(2)]
                    nmx = [sb(e0, "nmx%d" % i, [128, 1]) for i in range(2)]
                    ssm = [sb(e0, "ssm%d" % i, [128, 1]) for i in range(2)]
                    GT = [sb(e0, "GT%d" % i, [128, 128], BF16) for i in range(2)]
                    for tt in range(16):
                        i2 = tt % 2
                        xt_, xf = xTt[i2], xTf[i2]
                        for hf in range(2):
                            pt = ps[hf]
                            for j in range(4):
                                k = hf * 4 + j
                                P.op("pe", lambda e, pt=pt, j=j, k=k, tt=tt: e.transpose(out=pt.t[:, j * 128:(j + 1) * 128], in_=X.t[:, tt, k * 128:(k + 1) * 128], identity=ident.t[:]),
                                     [X.b[tt]] + ident.b, pt.b)
                            copy("act", xt_.t[:, hf * 4:(hf + 1) * 4, :], pt.t[:].rearrange("p (j t) -> p j t", j=4), pt.b, xt_.b)
                            P.op("dve", lambda e, xf=xf, xt_=xt_, hf=hf, pt=pt: e.tensor_tensor(out=xf.t[:, hf * 4:(hf + 1) * 4, :], in0=pt.t[:].rearrange("p (j t) -> p j t", j=4), in1=xt_.t[:, hf * 4:(hf + 1) * 4, :], op=ALU.subtract), pt.b + xt_.b, xf.b)
                        P.op("pool", lambda e, tt=tt: e.tensor_copy(out=Xb.t[:, tt, :], in_=X.t[:, tt, :]), [X.b[tt]], Xb.b)
                        pl = ps[2 + i2]
                        l_, t_, m_, e_, n_, s_, g_ = lg[i2], t8[i2], mk[i2], ex[i2], nmx[i2], ssm[i2], GT[i2]
                        for k in range(8):
                            P.op("pe", lambda e, pl=pl, k=k, xt_=xt_: e.matmul(pl.t[:, 0:32], xt_.t[:, k, :], wrh.t[:, k, :], start=(k == 0), stop=False), xt_.b + wrh.b, pl.b)
                            P.op("pe", lambda e, pl=pl, k=k, xt_=xt_: e.matmul(pl.t[:, 0:32], xt_.t[:, k, :], wrl.t[:, k, :], start=False, stop=False), xt_.b + wrl.b, pl.b)
                            P.op("pe", lambda e, pl=pl, xf=xf, k=k: e.matmul(pl.t[:, 0:32], xf.t[:, k, :], wrh.t[:, k, :], start=False, stop=(k == 7)), xf.b + wrh.b, pl.b)
                        P.op("dve", lambda e, pl=pl, l_=l_: e.tensor_tensor(out=l_.t[:], in0=pl.t[:, 0:32], in1=br_bc.t[:], op=ALU.add), pl.b + br_bc.b, l_.b)
                        P.op("dve", lambda e, l_=l_, t_=t_: e.max(out=t_.t[:], in_=l_.t[:]), l_.b, t_.b)
                        P.op("dve", lambda e, l_=l_, t_=t_, m_=m_: e.tensor_scalar(out=m_.t[:], in0=l_.t[:], scalar1=t_.t[:, 3:4], scalar2=None, op0=ALU.is_ge), l_.b + t_.b, m_.b)
                        P.op("dve", lambda e, m_=m_, tt=tt: e.tensor_copy(out=maskb.t[:, tt, :], in_=m_.t[:]), m_.b, [maskb.b[tt]])
                        P.op("dve", lambda e, t_=t_, n_=n_: e.tensor_scalar(out=n_.t[:], in0=t_.t[:, 0:1], scalar1=-1.0, scalar2=None, op0=ALU.mult), t_.b, n_.b)
                        P.op("act", lambda e, l_=l_, e_=e_, n_=n_: e.activation(out=e_.t[:], in_=l_.t[:], func=AF.Exp, bias=n_.t[:, 0:1]), l_.b + n_.b, e_.b)
                        P.op("dve", lambda e, e_=e_, m_=m_, s_=s_: e.scalar_tensor_tensor(out=e_.t[:], in0=e_.t[:], scalar=1.0, in1=m_.t[:], op0=ALU.mult, op1=ALU.mult, accum_out=s_.t[:]), e_.b + m_.b, e_.b + s_.b)
                        P.op("dve", lambda e, s_=s_: e.reciprocal(out=s_.t[:], in_=s_.t[:]), s_.b, s_.b)
                        gp_ = Gp[i2]
                        P.op("dve", lambda e, e_=e_, s_=s_, gp_=gp_: e.tensor_scalar(out=gp_.t[:, 0:32], in0=e_.t[:], scalar1=s_.t[:, 0:1], scalar2=None, op0=ALU.mult), e_.b + s_.b + gp_.b, gp_.b)
                        P.op("dve", lambda e, tt=tt, gp_=gp_: e.tensor_scalar(out=Ga.t[:, tt, :], in0=gp_.t[:, 0:32], scalar1=1.0 / SW_A, scalar2=None, op0=ALU.mult), gp_.b, Ga.b)
                        pp = ps[6 + i2]
                        g0 = (tt // 4) * 4
                        for t2 in range(g0, tt + 1):
                            P.op("pe", lambda e, pp=pp, t2=t2, tt=tt, g0=g0: e.matmul(pp.t[:, 0:32], (umat if t2 == tt else ones_b).t[:], maskb.t[:, t2, :], start=(t2 == g0), stop=(t2 == tt)),
                                 [maskb.b[t2]] + umat.b + ones_b.b, pp.b)
                        P.op("dve", lambda e, pp=pp, m_=m_, tt=tt: e.scalar_tensor_tensor(out=POS.t[:, tt, :], in0=pp.t[:, 0:32], scalar=1.0, in1=m_.t[:], op0=ALU.add, op1=ALU.mult), pp.b + m_.b, POS.b)
                        P.op("dve", lambda e, tt=tt: e.tensor_scalar(out=POS.t[:, tt, :], in0=POS.t[:, tt, :], scalar1=-1.0, scalar2=None, op0=ALU.add), POS.b, POS.b)
                        pg_ = ps[4 + i2]
                        P.op("pe", lambda e, pg_=pg_, gp_=gp_: e.transpose(out=pg_.t[:, 0:128], in_=gp_.t[:], identity=ident.t[:]), gp_.b + ident.b, pg_.b)
                        P.op("dve", lambda e, pg_=pg_, g_=g_: e.tensor_copy(out=g_.t[:], in_=pg_.t[:, 0:128]), pg_.b, g_.b)
                        for dh in range(2):
                            pb_ = ps[4 + i2] if dh == 0 else ps[2 + i2]
                            P.op("pe", lambda e, pb_=pb_, g_=g_, dh=dh: e.matmul(pb_.t[:], g_.t[:], bdnb.t[:, dh * 512:(dh + 1) * 512], start=True, stop=True), g_.b + bdnb.b, pb_.b)
                            P.op("dve", lambda e, pb_=pb_, tt=tt, dh=dh: e.scalar_tensor_tensor(out=X.t[:, tt, dh * 512:(dh + 1) * 512], in0=X.t[:, tt, dh * 512:(dh + 1) * 512], scalar=ALPHA, in1=pb_.t[:], op0=ALU.mult, op1=ALU.add),
                                 [X.b[tt]] + pb_.b, [X.b[tt]])
                    P.barrier()
                with contextlib.ExitStack() as e2:
                  if stop != "e0":
                    NW = 2
                    Wg = [sb(e2, "Wg%d" % i, [128, 8, 512], BF16) for i in range(NW)]
                    Wu = [sb(e2, "Wu%d" % i, [128, 8, 512], BF16) for i in range(NW)]
                    Wd = [sb(e2, "Wd%d" % i, [128, 4, 1024], BF16) for i in range(NW)]
                    gc = [sb(e2, "gc%d" % i, [128, 512]) for i in range(2)]
                    sl = [sb(e2, "sl%d" % i, [128, 512], BF16) for i in range(2)]
                    uc = [sb(e2, "uc%d" % i, [128, 512]) for i in range(2)]
                    AT = sb(e2, "AT", [128, 4, 512], BF16, nb=4)
                    Sel = [sb(e2, "Sel%d" % i, [128, 4, 128], BF16) for i in range(1)] * 2
                    Selg = [sb(e2, "Selg%d" % i, [128, 4, 128]) for i in range(1)] * 2
                    SelT2 = [sb(e2, "SelT%d" % i, [128, 16, 128], BF16, nb=4) for i in range(2)]
                    xeT = sb(e2, "xeT", [128, 8, 512], BF16, nb=4)
                    Yb = [sb(e2, "Yb%d" % i, [128, 4, 1024], BF16) for i in range(2)]
                    units = [(e_, hf) for e_ in range(n_experts) for hf in range(2)]

                    def load_unit(ui):
                        e_, hf = units[ui]
                        wi = ui % NW
                        gv = D["w_gu"][l, e_].rearrange("(k p) n -> p k n", p=128)
                        dv = D["w_down"][l, e_, hf * 512:(hf + 1) * 512, :].rearrange("(j p) n -> p j n", p=128)
                        for k0 in (0, 4):
                            P.dma("pool", lambda e, k0=k0: e.dma_start(out=Wg[wi].t[:, k0:k0 + 4, :], in_=gv[:, k0:k0 + 4, hf * 512:(hf + 1) * 512]), writes=Wg[wi].b)
                        for k0 in (0, 4):
                            P.dma("pool", lambda e, k0=k0: e.dma_start(out=Wu[wi].t[:, k0:k0 + 4, :], in_=gv[:, k0:k0 + 4, 1024 + hf * 512:1024 + (hf + 1) * 512]), writes=Wu[wi].b)
                        for j0 in (0, 2):
                            P.dma("pool", lambda e, j0=j0: e.dma_start(out=Wd[wi].t[:, j0:j0 + 2, :], in_=dv[:, j0:j0 + 2, :]), writes=Wd[wi].b)

                    load_unit(0)
                    load_unit(1)
                    cnt = 0
                    def scatter(e_):
                        SelT = SelT2[e_ % 2]
                        for tt in range(16):
                            grp = tt // 4
                            for dh in range(2):
                                py = ps[(tt * 2 + dh) % 4]
                                for hf in range(2):
                                    P.op("pe", lambda e, py=py, tt=tt, grp=grp, dh=dh, hf=hf: e.matmul(py.t[:], SelT.t[:, tt, :], Yb[hf].t[:, grp, dh * 512:(dh + 1) * 512], start=(hf == 0), stop=(hf == 1)), [SelT.b[grp]] + Yb[hf].b, py.b)
                                P.op("dve", lambda e, py=py, tt=tt, dh=dh: e.tensor_tensor(out=X.t[:, tt, dh * 512:(dh + 1) * 512], in0=py.t[:], in1=X.t[:, tt, dh * 512:(dh + 1) * 512], op=ALU.add),
                                     py.b + [X.b[tt]], [X.b[tt]])

                    for e_ in range(n_experts):
                        SelT = SelT2[e_ % 2]
                        for grp in range(4):
                            s_, sg_ = Sel[grp % 2], Selg[grp % 2]
                            for t4 in range(4):
                                tt = grp * 4 + t4
                                P.op("dve", lambda e, s_=s_, t4=t4, tt=tt, e_=e_: e.tensor_scalar(out=s_.t[:, t4, :], in0=iosl.t[:], scalar1=POS.t[:, tt, e_:e_ + 1], scalar2=None, op0=ALU.is_equal), iosl.b + POS.b, s_.b)
                                P.op("dve", lambda e, sg_=sg_, t4=t4, tt=tt, e_=e_: e.tensor_scalar(out=sg_.t[:, t4, :], in0=iosl.t[:], scalar1=POS.t[:, tt, e_:e_ + 1], scalar2=Ga.t[:, tt, e_:e_ + 1], op0=ALU.is_equal, op1=ALU.mult), iosl.b + POS.b + Ga.b, sg_.b)
                            for kh in range(2):
                                pgt = ps[kh]
                                for j in range(4):
                                    k = kh * 4 + j
                                    for t4 in range(4):
                                        tt = grp * 4 + t4
                                        P.op("pe", lambda e, pgt=pgt, j=j, k=k, t4=t4, tt=tt, s_=s_: e.matmul(pgt.t[:, j * 128:(j + 1) * 128], Xb.t[:, tt, k * 128:(k + 1) * 128], s_.t[:, t4, :], start=(t4 == 0), stop=(t4 == 3)),
                                             Xb.b + s_.b, pgt.b)
                                copy("act", xeT.t[:, kh * 4:(kh + 1) * 4, grp * 128:(grp + 1) * 128], pgt.t[:].rearrange("p (j t) -> p j t", j=4), pgt.b, [xeT.b[grp]])
                            ptr = ps[2 + grp % 2]
                            for t4 in range(4):
                                P.op("pe", lambda e, ptr=ptr, t4=t4, sg_=sg_: e.transpose(out=ptr.t[:, t4 * 128:(t4 + 1) * 128], in_=sg_.t[:, t4, :], identity=ident.t[:]), sg_.b + ident.b, ptr.b)
                            copy("act", SelT.t[:, grp * 4:(grp + 1) * 4, :], ptr.t[:].rearrange("p (j t) -> p j t", j=4), ptr.b, [SelT.b[grp]])
                        if e_ > 0:
                            scatter(e_ - 1)
                        for hf in range(2):
                            ui = e_ * 2 + hf
                            wi = ui % NW
                            yb_ = Yb[hf]
                            for j in range(4):
                                i2 = cnt % 2
                                cnt += 1
                                pg, pu = ps[4 + i2 * 2], ps[5 + i2 * 2]
                                g_, sl_, u_ = gc[i2], sl[i2], uc[i2]
                                cg = hf * 4 + j
                                cu = 8 + hf * 4 + j
                                for k in range(8):
                                    P.op("pe", lambda e, pg=pg, k=k, j=j, wi=wi: e.matmul(pg.t[:], Wg[wi].t[:, k, j * 128:(j + 1) * 128], xeT.t[:, k, :], start=(k == 0), stop=(k == 7)), Wg[wi].b + xeT.b, pg.b)
                                for k in range(8):
                                    P.op("pe", lambda e, pu=pu, k=k, j=j, wi=wi: e.matmul(pu.t[:], Wu[wi].t[:, k, j * 128:(j + 1) * 128], xeT.t[:, k, :], start=(k == 0), stop=(k == 7)), Wu[wi].b + xeT.b, pu.b)
                                P.op("dve", lambda e, pg=pg, g_=g_, cg=cg, e_=e_: e.tensor_scalar(out=g_.t[:], in0=pg.t[:], scalar1=bgu.t[:, cg, e_:e_ + 1], scalar2=SW_L, op0=ALU.add, op1=ALU.min), pg.b + bgu.b, g_.b)
                                P.op("act", lambda e, g_=g_, sl_=sl_: e.activation(out=sl_.t[:], in_=g_.t[:], func=AF.Silu, scale=SW_A), g_.b, sl_.b)
                                P.op("dve", lambda e, pu=pu, u_=u_, cu=cu, e_=e_: e.tensor_scalar(out=u_.t[:], in0=pu.t[:], scalar1=bgu.t[:, cu, e_:e_ + 1], scalar2=SW_L, op0=ALU.add, op1=ALU.min), pu.b + bgu.b, u_.b)
                                P.op("dve", lambda e, u_=u_: e.tensor_scalar(out=u_.t[:], in0=u_.t[:], scalar1=-SW_L, scalar2=1.0, op0=ALU.max, op1=ALU.add), u_.b, u_.b)
                                P.op("dve", lambda e, sl_=sl_, u_=u_, j=j: e.tensor_tensor(out=AT.t[:, j, :], in0=sl_.t[:], in1=u_.t[:], op=ALU.mult), sl_.b + u_.b, [AT.b[j]])
                            for s4 in range(4):
                                for dh in range(2):
                                    py = ps[(s4 * 2 + dh) % 4]
                                    for j in range(4):
                                        P.op("pe", lambda e, py=py, j=j, s4=s4, dh=dh, wi=wi: e.matmul(py.t[:], AT.t[:, j, s4 * 128:(s4 + 1) * 128], Wd[wi].t[:, j, dh * 512:(dh + 1) * 512], start=(j == 0), stop=(j == 3)), [AT.b[j]] + Wd[wi].b, py.b)
                                    copy("act", yb_.t[:, s4, dh * 512:(dh + 1) * 512], py.t[:], py.b, yb_.b)
                            if ui + 2 < len(units):
                                load_unit(ui + 2)
                    scatter(n_experts - 1)
                    P.barrier()
                with contextlib.ExitStack() as e3:
                  if stop not in ("e0", "e2"):
                    gbc = sb(e3, "gbce", [128, 1024]); bbc = sb(e3, "bbce", [128, 1024])
                    bcast_load(gbc, D["ln_g"][l, 2:3, :], 1024)
                    bcast_load(bbc, D["ln_b"][l, 2:3, :], 1024)
                    tmps = ln_tmps(e3, "e")
                    for tt in range(16):
                        ln_tile(e3, X.t[:, tt, :], [X.b[tt]], tt, gbc, bbc, tmps[tt % 2])
                    P.barrier()
            if stop in ("moe%d" % l, "e0", "e2"):
                break

        ov = out_d.rearrange("(tt p) d -> p tt d", p=128)
        for q4 in range(4):
            P.dma("sp", lambda e, q4=q4: e.dma_start(out=ov[:, q4 * 4:(q4 + 1) * 4, :], in_=X.t[:, q4 * 4:(q4 + 1) * 4, :]),
                  reads=X.b[q4 * 4:(q4 + 1) * 4], writes=[Buf()], is_output=True)
        with nc.allow_non_contiguous_dma(reason="small parameter loads"):
            P.emit()
    return nc


def kernel(**inputs):
    n = 8
    nc = build()
    x = np.ascontiguousarray(np.asarray(inputs["x"], dtype=np.float32))
    mem = np.ascontiguousarray(np.asarray(inputs["mem"], dtype=np.float32))
    ws = {k: np.ascontiguousarray(np.asarray(inputs[k], dtype=np.float32)) for k in W_NAMES}
    in_maps = []
    for b in range(n):
        m = {"x": x[b], "mem": mem[b]}
        m.update(ws)
        in_maps.append(m)
    res = run_bass_kernel_spmd(nc, in_maps, core_ids=list(range(n)))
    return np.stack([np.asarray(r["out"]) for r in res.results], axis=0).astype(np.float32)
```

```python
import contextlib
import numpy as np
import concourse.bass as bass
import concourse.mybir as mybir
from concourse.bass_utils import run_bass_kernel_spmd

F32 = mybir.dt.float32
BF16 = mybir.dt.bfloat16
I32 = mybir.dt.int32
AF = mybir.ActivationFunctionType
ALU = mybir.AluOpType

ENGS = ("pe", "act", "dve", "pool", "sp")
DEPTH = 2
ALPHA = float((2 * DEPTH) ** 0.25)
LN_EPS = 1e-5
RMS_EPS = 1e-6
SW_A = 1.702
SW_L = 7.0


class Buf:
    __slots__ = ("name", "last_w", "readers")

    def __init__(self, name=""):
        self.name = name
        self.last_w = None
        self.readers = []


class Op:
    __slots__ = ("eng", "fn", "is_dma", "pos", "waits", "dma_waits", "marked", "tick",
                 "dsem", "dval", "pre_dma_wait")

    def __init__(self, eng, fn, is_dma):
        self.eng = eng
        self.fn = fn
        self.is_dma = is_dma
        self.pos = -1
        self.waits = {}
        self.dma_waits = []
        self.marked = False
        self.tick = 0
        self.dsem = None
        self.dval = 0
        self.pre_dma_wait = None


class _Rec:
    def __init__(self):
        self.call = None

    def __getattr__(self, name):
        def f(*a, **k):
            self.call = (name, a, k)
            return self
        return f


class Prog:
    def __init__(self, nc, n_dma_sems=8):
        self.nc = nc
        self.ops = {e: [] for e in ENGS}
        self.seen = {e: {p: -1 for p in ENGS} for e in ENGS}
        self.seen_dma = {e: set() for e in ENGS}
        self.n_dma_sems = n_dma_sems
        self.dma_count = {e: 0 for e in ENGS}
        self.dma_hist = {e: [] for e in ENGS}
        self.pending_dmas = []
        self.all_dma_out = []

    def _add_dep(self, op, dep):
        if dep is None or dep is op:
            return
        e = op.eng
        if dep.is_dma:
            if dep in self.seen_dma[e]:
                return
            self.seen_dma[e].add(dep)
            op.dma_waits.append(dep)
            return
        if dep.eng == e and e == "pe" and not op.is_dma:
            return
        if self.seen[e][dep.eng] >= dep.pos:
            return
        cur = op.waits.get(dep.eng)
        if cur is None or cur.pos < dep.pos:
            op.waits[dep.eng] = dep

    def _finish(self, op):
        eng = op.eng
        for pe_, d in op.waits.items():
            self.seen[eng][pe_] = max(self.seen[eng][pe_], d.pos)
            d.marked = True
        self.ops[eng].append(op)

    def _record(self, eng, fn, reads, writes, is_dma):
        rec = _Rec()
        fn(rec)
        name_, a_, k_ = rec.call
        fn = (lambda e, name_=name_, a_=a_, k_=k_: getattr(e, name_)(*a_, **k_))
        op = Op(eng, fn, is_dma)
        op.pos = len(self.ops[eng])
        for b in reads:
            self._add_dep(op, b.last_w)
        for b in writes:
            self._add_dep(op, b.last_w)
            for r in b.readers:
                self._add_dep(op, r)
        for b in reads:
            b.readers.append(op)
        for b in writes:
            b.last_w = op
            b.readers = []
        if is_dma:
            k = self.dma_count[eng]
            self.dma_count[eng] += 1
            op.dsem = k % self.n_dma_sems
            op.dval = 16 * (k // self.n_dma_sems + 1)
            hist = self.dma_hist[eng]
            if k >= self.n_dma_sems:
                prev = hist[k - self.n_dma_sems]
                if prev not in self.seen_dma[eng]:
                    self.seen_dma[eng].add(prev)
                    op.pre_dma_wait = prev
            hist.append(op)
            self.pending_dmas.append(op)
        self._finish(op)
        return op

    def op(self, eng, fn, reads=(), writes=()):
        return self._record(eng, fn, reads, writes, False)

    def dma(self, eng, fn, reads=(), writes=(), is_output=False):
        o = self._record(eng, fn, reads, writes, True)
        if is_output:
            self.all_dma_out.append(o)
        return o

    def _last_compute(self, e):
        for o in reversed(self.ops[e]):
            if not o.is_dma and o.fn is not None:
                return o
        return None

    def barrier(self):
        scr = self.bar_scratch
        op = Op("dve", lambda e, scr=scr: e.memset(scr[0:1, 0:1], 0.0), False)
        op.pos = len(self.ops["dve"])
        for p in ENGS:
            d = self._last_compute(p)
            if d is not None:
                self._add_dep(op, d)
        for d in self.pending_dmas:
            self._add_dep(op, d)
        self.pending_dmas = []
        self._finish(op)
        for e in ENGS:
            if e == "dve":
                continue
            w = Op(e, None, False)
            w.pos = len(self.ops[e])
            self._add_dep(w, op)
            self._finish(w)

    def emit(self):
        nc = self.nc
        engmap = {"pe": "tensor", "act": "scalar", "dve": "vector", "pool": "gpsimd", "sp": "sync"}
        final = Op("sp", None, False)
        final.pos = len(self.ops["sp"])
        for d in self.all_dma_out:
            if d not in self.seen_dma["sp"]:
                final.dma_waits.append(d)
        for e in ENGS:
            t = 0
            for o in self.ops[e]:
                if o.marked and not o.is_dma:
                    t += 1
                o.tick = t
        with contextlib.ExitStack() as st:
            sems = {e: st.enter_context(nc.semaphore("s_" + e)) for e in ENGS}
            dsems = {e: [st.enter_context(nc.semaphore("d_%s_%d" % (e, i)))
                         for i in range(self.n_dma_sems)] for e in ("sp", "act", "pool")}
            block = st.enter_context(nc.Block())

            def make(e):
                def body(eng):
                    lst = list(self.ops[e])
                    if e == "sp":
                        lst = lst + [final]
                    for o in lst:
                        for pe_, d in o.waits.items():
                            eng.wait_ge(sems[pe_], d.tick)
                        for d in o.dma_waits:
                            eng.wait_ge(dsems[d.eng][d.dsem], d.dval)
                        if o.pre_dma_wait is not None:
                            d = o.pre_dma_wait
                            eng.wait_ge(dsems[d.eng][d.dsem], d.dval)
                        if o.fn is None:
                            continue
                        ins = o.fn(eng)
                        if o.is_dma:
                            ins.then_inc(dsems[e][o.dsem], 16)
                        elif o.marked:
                            ins.then_inc(sems[e], 1)
                return body

            for e in ENGS:
                getattr(block, engmap[e])(make(e))


class T:
    def __init__(self, t, nb=1):
        self.t = t
        self.b = [Buf() for _ in range(nb)]


W_NAMES = ["w_in", "sgu_g", "sgu_b", "w_sp", "b_sp", "grp_g", "w_out", "wq_mem", "wkv_mem", "wo_mem",
           "w_router", "b_router", "w_gu", "b_gu", "w_down", "b_down", "ln_g", "ln_b"]
W_SHAPES = {
    "w_in": [2, 1024, 2560], "sgu_g": [2, 512], "sgu_b": [2, 512], "w_sp": [2, 4, 128, 128],
    "b_sp": [2, 4, 128], "grp_g": [2, 1024], "w_out": [2, 1024, 1024], "wq_mem": [2, 1024, 1024],
    "wkv_mem": [2, 1024, 2048], "wo_mem": [2, 1024, 1024], "w_router": [2, 1024, 32],
    "b_router": [2, 32], "w_gu": [2, 32, 1024, 2048], "b_gu": [2, 32, 2048],
    "w_down": [2, 32, 1024, 1024], "b_down": [2, 32, 1024], "ln_g": [2, 3, 1024], "ln_b": [2, 3, 1024],
}


def build(stop=None, n_layers=DEPTH, n_experts=32):
    nc = bass.Bass("TRN2", target_bir_lowering=False)
    P = Prog(nc)
    D = {}
    D["x"] = nc.dram_tensor("x", [2048, 1024], F32, kind="ExternalInput").ap()
    D["mem"] = nc.dram_tensor("mem", [256, 1024], F32, kind="ExternalInput").ap()
    for n in W_NAMES:
        D[n] = nc.dram_tensor(n, W_SHAPES[n], F32, kind="ExternalInput").ap()
    out_d = nc.dram_tensor("out", [2048, 1024], F32, kind="ExternalOutput").ap()

    rr = [0]
    uid = [0]

    with contextlib.ExitStack() as top:
        def _sp():
            try:
                rr[0] += 1000
                nc.sbuf_tensor("zzprobe%d" % rr[0], [128, 60000], F32).__enter__()
            except BaseException as ex:
                import re as _re
                m_ = _re.search(r"base=(\d+)", str(ex))
                return int(m_.group(1)) if m_ else -1
            return -2

        def sb(st, name, shape, dt=F32, nb=1):
            import os as _os
            if False:
                b0 = _sp()
                t_ = T(st.enter_context(nc.sbuf_tensor(name, shape, dt)), nb)
                print("ALLOC", name, b0, _sp())
                return t_
            uid[0] += 1
            return T(st.enter_context(nc.sbuf_tensor("%s_%d" % (name, uid[0]), shape, dt)), nb)

        X = sb(top, "X", [128, 16, 1024], F32, nb=16)
        ps = [T(top.enter_context(nc.psum_tensor("ps%d" % i, [128, 512], F32))) for i in range(8)]
        P.bar_scratch = top.enter_context(nc.sbuf_tensor("barscr", [128, 8], F32))
        ident = sb(top, "ident", [128, 128])
        tmat = sb(top, "tmat", [128, 128], BF16)
        ones_b = sb(top, "ones_b", [128, 128], BF16)
        nones_b = sb(top, "nones_b", [128, 128], BF16)
        ntmat = sb(top, "ntmat", [128, 128], BF16)
        bd_f = sb(top, "bd_f", [128, 128])
        cmask = sb(top, "cmask", [128, 128])
        m01 = sb(top, "m01", [128, 896], BF16)
        mneg = sb(top, "mneg", [128, 896])
        umat = sb(top, "umat", [128, 128], BF16)
        iosl = sb(top, "iosl", [128, 128])
        cst = contextlib.ExitStack()
        iof = sb(cst, "iof", [128, 896])
        iop = sb(cst, "iop", [128, 1])
        ioi = sb(cst, "ioi", [128, 896], I32)
        iopi = sb(cst, "iopi", [128, 1], I32)

        def cp_engine():
            rr[0] += 1
            return "act" if rr[0] % 2 else "dve"

        def copy(eng, out, in_, reads, writes, scale=None):
            if eng == "act":
                if scale is None:
                    P.op("act", lambda e: e.activation(out=out, in_=in_, func=AF.Copy), reads, writes)
                else:
                    P.op("act", lambda e: e.activation(out=out, in_=in_, func=AF.Copy, scale=scale), reads, writes)
            else:
                if scale is None:
                    P.op(eng, lambda e: e.tensor_copy(out=out, in_=in_), reads, writes)
                else:
                    P.op(eng, lambda e: e.tensor_scalar(out=out, in0=in_, scalar1=scale, scalar2=None, op0=ALU.mult), reads, writes)

        P.op("pool", lambda e: e.iota(ioi.t[:], [[1, 896]], base=-384, channel_multiplier=0), writes=ioi.b)
        P.op("pool", lambda e: e.iota(iopi.t[:], [[0, 1]], base=0, channel_multiplier=1), writes=iopi.b)
        P.op("dve", lambda e: e.tensor_copy(out=iof.t[:], in_=ioi.t[:]), ioi.b, iof.b)
        P.op("dve", lambda e: e.tensor_copy(out=iop.t[:], in_=iopi.t[:]), iopi.b, iop.b)
        jf = iof.t[:, 384:512]
        cb = iof.b + iop.b
        P.op("dve", lambda e: e.tensor_scalar(out=ident.t[:], in0=jf, scalar1=iop.t[:, 0:1], scalar2=None, op0=ALU.is_equal), cb, ident.b)
        P.op("dve", lambda e: e.tensor_scalar(out=tmat.t[:], in0=jf, scalar1=iop.t[:, 0:1], scalar2=None, op0=ALU.is_lt), cb, tmat.b)
        P.op("dve", lambda e: e.tensor_scalar(out=cmask.t[:], in0=jf, scalar1=iop.t[:, 0:1], scalar2=None, op0=ALU.is_ge), cb, cmask.b)
        P.op("dve", lambda e: e.tensor_scalar(out=m01.t[:], in0=iof.t[:], scalar1=iop.t[:, 0:1], scalar2=None, op0=ALU.is_gt), cb, m01.b)
        P.op("dve", lambda e: e.tensor_scalar(out=mneg.t[:], in0=iof.t[:], scalar1=iop.t[:, 0:1], scalar2=None, op0=ALU.is_gt), cb, mneg.b)
        P.op("dve", lambda e: e.tensor_scalar(out=mneg.t[:], in0=mneg.t[:], scalar1=1.0, scalar2=30000.0, op0=ALU.subtract, op1=ALU.mult), mneg.b, mneg.b)
        P.op("pool", lambda e: e.memset(ones_b.t[:], 1.0), writes=ones_b.b)
        P.op("pool", lambda e: e.memset(nones_b.t[:], -1.0), writes=nones_b.b)
        P.op("dve", lambda e: e.tensor_scalar(out=ntmat.t[:], in0=jf, scalar1=iop.t[:, 0:1], scalar2=-1.0, op0=ALU.is_lt, op1=ALU.mult), cb, ntmat.b)
        P.op("dve", lambda e: e.tensor_scalar(out=umat.t[:], in0=jf, scalar1=iop.t[:, 0:1], scalar2=None, op0=ALU.is_gt), cb, umat.b)
        P.op("dve", lambda e: e.tensor_copy(out=iosl.t[:], in_=jf), cb, iosl.b)
        P.op("pool", lambda e: e.memset(bd_f.t[:], 0.0), writes=bd_f.b)
        P.op("pool", lambda e: e.memset(bd_f.t[0:64, 0:64], 1.0), bd_f.b, bd_f.b)
        P.op("pool", lambda e: e.memset(bd_f.t[64:128, 64:128], 1.0), bd_f.b, bd_f.b)

        P.barrier()
        cst.close()
        xv = D["x"].rearrange("(tt p) d -> p tt d", p=128)
        for q4 in range(4):
            P.dma("sp", lambda e, q4=q4: e.dma_start(out=X.t[:, q4 * 4:(q4 + 1) * 4, :], in_=xv[:, q4 * 4:(q4 + 1) * 4, :]),
                  writes=X.b[q4 * 4:(q4 + 1) * 4])

        def bcast_load(dst, src_row, n):
            P.dma("sp", lambda e: e.dma_start(out=dst.t[:, 0:n], in_=src_row.partition_broadcast(128)), writes=dst.b)

        def wload(dst, src, kchunks, c0, c1):
            v = src.rearrange("(k p) n -> p k n", p=128)
            step = max(1, 2048 // (c1 - c0))
            for k0 in range(0, kchunks, step):
                k1 = min(kchunks, k0 + step)
                P.dma("pool", lambda e, k0=k0, k1=k1: e.dma_start(out=dst.t[:, k0:k1, :], in_=v[:, k0:k1, c0:c1]),
                      writes=dst.b)

        def build_xT(xT, extra=None):
            for tt in range(16):
                for hf in range(2):
                    pt = ps[hf]
                    for j in range(4):
                        k = hf * 4 + j
                        P.op("pe", lambda e, pt=pt, j=j, k=k, tt=tt: e.transpose(out=pt.t[:, j * 128:(j + 1) * 128], in_=X.t[:, tt, k * 128:(k + 1) * 128], identity=ident.t[:]),
                             [X.b[tt]] + ident.b, pt.b)
                    copy(cp_engine(), xT.t[:, hf * 4:(hf + 1) * 4, tt * 128:(tt + 1) * 128],
                         pt.t[:].rearrange("p (j t) -> p j t", j=4), pt.b, xT.b)
                    if extra is not None:
                        extra(tt, hf, pt)

        def rsqrt_(ap, bufs):
            P.op("act", lambda e: e.activation(out=ap, in_=ap, func=AF.Ln), bufs, bufs)
            P.op("act", lambda e: e.activation(out=ap, in_=ap, func=AF.Exp, scale=-0.5), bufs, bufs)

        def ln_tile(st_, r, rb, tt, gbc, bbc, tmp):
            stt, mv, rs, nmr, xn = tmp
            P.op("dve", lambda e: e.bn_stats(out=stt.t[:, 0, :], in_=r[:, 0:512]), rb, stt.b)
            P.op("dve", lambda e: e.bn_stats(out=stt.t[:, 1, :], in_=r[:, 512:1024]), rb, stt.b)
            P.op("dve", lambda e: e.bn_aggr(out=mv.t[:], in_=stt.t[:].rearrange("p a b -> p (a b)")), stt.b, mv.b)
            P.op("dve", lambda e: e.tensor_scalar(out=rs.t[:], in0=mv.t[:, 1:2], scalar1=LN_EPS, scalar2=None, op0=ALU.add), mv.b, rs.b)
            rsqrt_(rs.t[:], rs.b)
            P.op("dve", lambda e: e.scalar_tensor_tensor(out=nmr.t[:], in0=mv.t[:, 0:1], scalar=-1.0, in1=rs.t[:], op0=ALU.mult, op1=ALU.mult), mv.b + rs.b, nmr.b)
            P.op("act", lambda e: e.activation(out=xn.t[:], in_=r, func=AF.Identity, bias=nmr.t[:, 0:1], scale=rs.t[:, 0:1]), rb + nmr.b + rs.b, xn.b)
            P.op("pool", lambda e: e.tensor_tensor(out=xn.t[:], in0=xn.t[:], in1=gbc.t[:], op=ALU.mult), xn.b + gbc.b, xn.b)
            P.op("dve", lambda e: e.tensor_tensor(out=X.t[:, tt, :], in0=xn.t[:], in1=bbc.t[:], op=ALU.add), xn.b + bbc.b, [X.b[tt]])

        def ln_tmps(st_, tag):
            return [(sb(st_, "lnst%s%d" % (tag, i), [128, 2, 6]), sb(st_, "lnmv%s%d" % (tag, i), [128, 2]),
                     sb(st_, "lnrs%s%d" % (tag, i), [128, 1]), sb(st_, "lnnm%s%d" % (tag, i), [128, 1]),
                     sb(st_, "lnxn%s%d" % (tag, i), [128, 1024])) for i in range(2)]

        def proj_ln(st_, srcT, w, l, li, tag):
            gbc = sb(st_, "gbc" + tag, [128, 1024]); bbc = sb(st_, "bbc" + tag, [128, 1024])
            bcast_load(gbc, D["ln_g"][l, li:li + 1, :], 1024)
            bcast_load(bbc, D["ln_b"][l, li:li + 1, :], 1024)
            tmps = ln_tmps(st_, tag)
            rbuf = [sb(st_, "r%s%d" % (tag, i), [128, 1024]) for i in range(2)]
            for tt in range(16):
                r = rbuf[tt % 2]
                for dh in range(2):
                    pm = ps[2 + (tt * 2 + dh) % 4]
                    for c in range(8):
                        P.op("pe", lambda e, pm=pm, c=c, tt=tt, dh=dh: e.matmul(pm.t[:], srcT.t[:, c, tt * 128:(tt + 1) * 128], w.t[:, c, dh * 512:(dh + 1) * 512], start=(c == 0), stop=(c == 7)),
                             srcT.b + w.b, pm.b)
                    P.op("dve", lambda e, pm=pm, r=r, tt=tt, dh=dh: e.scalar_tensor_tensor(out=r.t[:, dh * 512:(dh + 1) * 512], in0=X.t[:, tt, dh * 512:(dh + 1) * 512], scalar=ALPHA, in1=pm.t[:], op0=ALU.mult, op1=ALU.add),
                         [X.b[tt]] + pm.b, r.b)
                ln_tile(st_, r.t[:], r.b, tt, gbc, bbc, tmps[tt % 2])

        for l in range(n_layers):
            with contextlib.ExitStack() as mx:
                yT = sb(mx, "yT", [128, 8, 2048], BF16)
                grpc = sb(mx, "grpc", [128, 8])
                P.dma("sp", lambda e: e.dma_start(out=grpc.t[:], in_=D["grp_g"][l].rearrange("(c p) -> p c", p=128)), writes=grpc.b)
                with contextlib.ExitStack() as ab:
                    qT = sb(ab, "qT", [128, 4, 2048], BF16)
                    kT = sb(ab, "kT", [128, 4, 2048], BF16)
                    V = sb(ab, "V", [128, 16, 512], BF16)
                    with contextlib.ExitStack() as a1:
                        xT = sb(a1, "xT", [128, 8, 2048], BF16)
                        wq3 = [sb(a1, "wqkv%d" % i, [128, 8, 512], BF16) for i in range(2)]
                        wload(wq3[0], D["w_in"][l], 8, 1024, 1536)
                        wload(wq3[1], D["w_in"][l], 8, 1536, 2048)
                        build_xT(xT)
                        for c in range(8):
                            wp = wq3[c // 4]
                            for tb in range(4):
                                pm = ps[2 + (c * 4 + tb) % 4]
                                for k in range(8):
                                    P.op("pe", lambda e, pm=pm, c=c, tb=tb, k=k, wp=wp: e.matmul(pm.t[:], wp.t[:, k, (c % 4) * 128:(c % 4 + 1) * 128], xT.t[:, k, tb * 512:(tb + 1) * 512], start=(k == 0), stop=(k == 7)),
                                         wp.b + xT.b, pm.b)
                                if c < 4:
                                    copy(cp_engine(), qT.t[:, c, tb * 512:(tb + 1) * 512], pm.t[:], pm.b, qT.b, scale=0.125)
                                else:
                                    copy(cp_engine(), kT.t[:, c - 4, tb * 512:(tb + 1) * 512], pm.t[:], pm.b, kT.b)
                            if c == 3:
                                wload(wq3[0], D["w_in"][l], 8, 2048, 2560)
                        for tt in range(16):
                            pm = ps[2 + tt % 4]
                            for k in range(8):
                                P.op("pe", lambda e, pm=pm, tt=tt, k=k: e.matmul(pm.t[:], xT.t[:, k, tt * 128:(tt + 1) * 128], wq3[0].t[:, k, :], start=(k == 0), stop=(k == 7)),
                                     wq3[0].b + xT.b, pm.b)
                            copy(cp_engine(), V.t[:, tt, :], pm.t[:], pm.b, V.b)
                        P.barrier()
                    if stop == "a1q":
                        P.op("dve", lambda e: e.tensor_copy(out=X.t[:, 0:8, :].rearrange("p a b -> p (a b)"), in_=qT.t[:].rearrange("p a b -> p (a b)")), qT.b + X.b[0:8], X.b[0:8])
                        P.op("dve", lambda e: e.tensor_copy(out=X.t[:, 8:16, :].rearrange("p a b -> p (a b)"), in_=kT.t[:].rearrange("p a b -> p (a b)")), kT.b + X.b[8:16], X.b[8:16])
                        P.barrier()
                    with contextlib.ExitStack() as bb:
                      if stop not in ("a1", "a1q"):
                        NB = 2
                        Esb = [sb(bb, "Esb%d" % i, [128, 512]) for i in range(NB)]
                        Lb = [sb(bb, "Lb%d" % i, [128, 512], BF16) for i in range(4)]
                        tmp = [sb(bb, "tmpb%d" % i, [128, 512]) for i in range(NB)]
                        aT = [sb(bb, "aT%d" % i, [128, 512], BF16) for i in range(NB)]
                        accb = [sb(bb, "accb%d" % i, [128, 2048], BF16, nb=4) for i in range(1)] * 2
                        outT = [sb(bb, "outT%d" % i, [128, 512]) for i in range(2)]
                        sq = [sb(bb, "sq%d" % i, [128, 512]) for i in range(1)] * 2
                        rsd = [sb(bb, "rsd%d" % i, [128, 512]) for i in range(1)] * 2
                        kz = [sb(bb, "kz%d" % i, [128, 2048], BF16) for i in range(2)]
                        Vz = [sb(bb, "Vz%d" % i, [128, 16, 128], BF16) for i in range(2)]
                        for i in range(2):
                            P.op("pool", lambda e, i=i: e.memset(kz[i].t[:], 0.0), writes=kz[i].b)
                            P.op("pool", lambda e, i=i: e.memset(Vz[i].t[:], 0.0), writes=Vz[i].b)
                        un = 0
                        import os
                        BK = 0; BHP = 4; BEPI = 1; BC = 0; BKMAX = 15
                        qA = []
                        qB = []
                        qC = []

                        def drainA():
                            cv, f1, fb, fc = qA.pop(0)
                            f1()
                            qB.append((cv, fb, fc))

                        def drainB():
                            cv, fb, fc = qB.pop(0)
                            while any(pc == cv for pc, _ in qC):
                                qC.pop(0)[1]()
                            fb()
                            qC.append((cv, fc))

                        def make_unit(hp, hi, kb, c, un, ac):
                            diag = (c == kb // 4)
                            r_ = kb % 4
                            o0 = 384 - 128 * r_
                            first = (kb == 4 * c + 3)
                            zp = ps[4 + un % 4]
                            op_ = ps[c]
                            E_, L_, t_, a_ = Esb[un % NB], Lb[un % 4], tmp[un % NB], aT[un % NB]

                            def s0():
                                P.op("pe", lambda e: e.matmul(zp.t[:], kz[hi].t[:, kb * 128:(kb + 1) * 128], qT.t[:, hp, c * 512:(c + 1) * 512], start=True, stop=True),
                                     kz[hi].b + qT.b, zp.b)

                            def s1():
                                P.op("act", lambda e: e.activation(out=E_.t[:], in_=zp.t[:], func=AF.Exp), zp.b, E_.b)
                                P.op("act", lambda e: e.activation(out=L_.t[:], in_=E_.t[:], func=AF.Ln, bias=1.0), E_.b, L_.b)
                                if diag:
                                    P.op("pool", lambda e: e.tensor_tensor(out=L_.t[:], in0=L_.t[:], in1=m01.t[:, o0:o0 + 512], op=ALU.mult), L_.b + m01.b, L_.b)

                            def s1b():
                                P.op("pe", lambda e: e.matmul(zp.t[:], ntmat.t[:], L_.t[:], start=False, stop=first, skip_group_check=True), L_.b + ntmat.b, zp.b)
                                if not first:
                                    P.op("pe", lambda e: e.matmul(zp.t[:], nones_b.t[:], ac.t[:, c * 512:(c + 1) * 512], start=False, stop=True, skip_group_check=True), [ac.b[c]] + nones_b.b, zp.b)

                            def s2():
                                P.op("dve", lambda e: e.tensor_tensor(out=t_.t[:], in0=zp.t[:], in1=L_.t[:], op=ALU.subtract), zp.b + L_.b, t_.b)
                                if diag:
                                    P.op("dve", lambda e: e.tensor_tensor(out=t_.t[:], in0=t_.t[:], in1=mneg.t[:, o0:o0 + 512], op=ALU.add), t_.b + mneg.b, t_.b)
                                P.op("act", lambda e: e.activation(out=a_.t[:], in_=t_.t[:], func=AF.Exp), t_.b, a_.b)
                                P.op("pe", lambda e: e.matmul(op_.t[:], Vz[hi].t[:, kb, :], a_.t[:], start=(first and hi == 0), stop=(kb == 0 and hi == 1)),
                                     Vz[hi].b + a_.b, op_.b)
                                if kb > 0:
                                    if first:
                                        P.op("pool", lambda e: e.tensor_copy(out=ac.t[:, c * 512:(c + 1) * 512], in_=L_.t[:]), L_.b, [ac.b[c]])
                                    else:
                                        P.op("pool", lambda e: e.tensor_tensor(out=ac.t[:, c * 512:(c + 1) * 512], in0=ac.t[:, c * 512:(c + 1) * 512], in1=L_.t[:], op=ALU.add), L_.b + [ac.b[c]], [ac.b[c]])
                            return s0, s1, s1b, s2

                        for hp in range(BHP):
                            for hi in range(2):
                                h = hp * 2 + hi
                                hs = hi * 64
                                ac = accb[hi]
                                P.op("pool", lambda e, hi=hi, hs=hs, hp=hp: e.tensor_copy(out=kz[hi].t[hs:hs + 64, :], in_=kT.t[hs:hs + 64, hp, :]), kT.b + kz[hi].b, kz[hi].b)
                                P.op("pool", lambda e, hi=hi, hs=hs, h=h: e.tensor_copy(out=Vz[hi].t[:, :, hs:hs + 64], in_=V.t[:, :, h * 64:(h + 1) * 64]), V.b + Vz[hi].b, Vz[hi].b)
                                for kb in range(BKMAX, BK - 1, -1):
                                    for c in range(3, max(BC, kb // 4) - 1, -1):
                                        s0, s1, s1b, s2 = make_unit(hp, hi, kb, c, un, ac)
                                        un += 1
                                        s0()
                                        qA.append((c, s1, s1b, s2))
                                        while len(qA) > 1:
                                            drainA()
                                        while len(qB) > 1:
                                            drainB()
                                        while len(qC) > 1:
                                            qC.pop(0)[1]()
                            while qA:
                                drainA()
                            while qB:
                                drainB()
                            while qC:
                                qC.pop(0)[1]()
                            if None == '1' and hp == 0:
                                for c in range(4):
                                    P.op("dve", lambda e, c=c: e.tensor_copy(out=X.t[:, c, 0:512], in_=ps[c].t[:]), ps[c].b + [X.b[c]], [X.b[c]])
                            EPS = 9; EPC0 = 0
                            for c in range(EPC0, 4 if BEPI else 0):
                                op_ = ps[c]
                                sq_, rs_ = sq[c % 2], rsd[c % 2]
                                sp_ = ps[4 + c % 2]
                                P.op("dve", lambda e, op_=op_, c=c: e.tensor_copy(out=outT[c % 2].t[:], in_=op_.t[:]), op_.b, outT[c % 2].b)
                                if EPS >= 1:
                                    P.op("dve", lambda e, c=c, sq_=sq_: e.tensor_tensor(out=sq_.t[:], in0=outT[c % 2].t[:], in1=outT[c % 2].t[:], op=ALU.mult), outT[c % 2].b, sq_.b)
                                if EPS >= 2:
                                    P.op("pe", lambda e, sp_=sp_, sq_=sq_: e.matmul(sp_.t[:], bd_f.t[:], sq_.t[:], start=True, stop=True), sq_.b + bd_f.b, sp_.b)
                                if EPS >= 3:
                                    P.op("dve", lambda e, sp_=sp_, rs_=rs_: e.tensor_scalar(out=rs_.t[:], in0=sp_.t[:], scalar1=1.0 / 64.0, scalar2=RMS_EPS, op0=ALU.mult, op1=ALU.add), sp_.b, rs_.b)
                                if EPS >= 4:
                                    rsqrt_(rs_.t[:], rs_.b)
                                if None == '2' and hp == 0:
                                    P.op("dve", lambda e, c=c: e.tensor_copy(out=X.t[:, 4 + c, 0:512], in_=outT[c % 2].t[:]), outT[c % 2].b + [X.b[4 + c]], [X.b[4 + c]])
                                    P.op("dve", lambda e, c=c, rs_=rs_: e.tensor_copy(out=X.t[:, 8 + c, 0:512], in_=rs_.t[:]), rs_.b + [X.b[8 + c]], [X.b[8 + c]])
                                    P.op("dve", lambda e, c=c, sq_=sq_: e.tensor_copy(out=X.t[:, 12 + c, 0:512], in_=sq_.t[:]), sq_.b + [X.b[12 + c]], [X.b[12 + c]])
                                    P.op("dve", lambda e, c=c: e.tensor_copy(out=X.t[:, 12 + c, 512:520], in_=grpc.t[:]), grpc.b + [X.b[12 + c]], [X.b[12 + c]])
                                if None == '3' and hp == 0:
                                    P.op("dve", lambda e, rs_=rs_, c=c, hp=hp: e.scalar_tensor_tensor(out=yT.t[:, 4 + hp, c * 512:(c + 1) * 512], in0=outT[c % 2].t[:], scalar=grpc.t[:, 4 + hp:5 + hp], in1=rs_.t[:], op0=ALU.mult, op1=ALU.mult),
                                         outT[c % 2].b + rs_.b + grpc.b, yT.b)
                                    P.op("dve", lambda e, c=c: e.tensor_copy(out=X.t[:, 4 + c, 0:512], in_=outT[c % 2].t[:]), outT[c % 2].b + [X.b[4 + c]], [X.b[4 + c]])
                                    P.op("dve", lambda e, c=c, rs_=rs_: e.tensor_copy(out=X.t[:, 8 + c, 0:512], in_=rs_.t[:]), rs_.b + [X.b[8 + c]], [X.b[8 + c]])
                                    P.op("dve", lambda e, c=c, hp=hp: e.tensor_copy(out=X.t[:, 12 + c, 0:512], in_=yT.t[:, 4 + hp, c * 512:(c + 1) * 512]), yT.b + [X.b[12 + c]], [X.b[12 + c]])
                                    P.op("dve", lambda e, c=c: e.tensor_copy(out=X.t[:, 12 + c, 512:520], in_=grpc.t[:]), grpc.b + [X.b[12 + c]], [X.b[12 + c]])
                                    continue
                                if EPS >= 5:
                                    P.op("dve", lambda e, rs_=rs_, c=c, hp=hp: e.scalar_tensor_tensor(out=yT.t[:, 4 + hp, c * 512:(c + 1) * 512], in0=outT[c % 2].t[:], scalar=grpc.t[:, 4 + hp:5 + hp], in1=rs_.t[:], op0=ALU.mult, op1=ALU.mult),
                                         outT[c % 2].b + rs_.b + grpc.b, yT.b)
                        P.barrier()
                with contextlib.ExitStack() as a2:
                  if stop not in ("a1", "a1q", "b", "by"):
                    xT = sb(a2, "xT2", [128, 8, 2048], BF16)
                    wuv = sb(a2, "wuv", [128, 8, 1024], BF16)
                    wload(wuv, D["w_in"][l], 8, 0, 1024)
                    wsp = sb(a2, "wsp", [128, 4, 128])
                    wcT = sb(a2, "wcT", [128, 4, 128], BF16)
                    bsp = sb(a2, "bsp", [128, 4])
                    sg_bc = sb(a2, "sg_bc", [128, 512]); sb_bc = sb(a2, "sb_bc", [128, 512]); gg_bc = sb(a2, "gg_bc", [128, 512])
                    bcast_load(sg_bc, D["sgu_g"][l:l + 1, :], 512)
                    bcast_load(sb_bc, D["sgu_b"][l:l + 1, :], 512)
                    bcast_load(gg_bc, D["grp_g"][l:l + 1, 0:512], 512)
                    P.dma("sp", lambda e: e.dma_start(out=wsp.t[:], in_=D["w_sp"][l].rearrange("g t s -> t g s")), writes=wsp.b)
                    P.dma("sp", lambda e: e.dma_start(out=bsp.t[:], in_=D["b_sp"][l].rearrange("g t -> t g")), writes=bsp.b)
                    for g in range(4):
                        P.op("pe", lambda e, g=g: e.transpose(out=ps[0].t[:, g * 128:(g + 1) * 128], in_=wsp.t[:, g, :], identity=ident.t[:]), wsp.b + ident.b, ps[0].b)
                    for g in range(4):
                        P.op("dve", lambda e, g=g: e.tensor_tensor(out=wcT.t[:, g, :], in0=ps[0].t[:, g * 128:(g + 1) * 128], in1=cmask.t[:], op=ALU.mult), ps[0].b + cmask.b, wcT.b)
                    build_xT(xT)
                    ug = [sb(a2, "ug%d" % i, [128, 512]) for i in range(2)]
                    vg = [sb(a2, "vg%d" % i, [128, 512]) for i in range(2)]
                    vnb = [sb(a2, "vnb%d" % i, [128, 512], BF16) for i in range(2)]
                    oa = [sb(a2, "oa%d" % i, [128, 512]) for i in range(2)]
                    junk = [sb(a2, "junk%d" % i, [128, 128]) for i in range(2)]
                    st4 = [sb(a2, "st4%d" % i, [128, 4, 6]) for i in range(2)]
                    mv4 = [sb(a2, "mv4%d" % i, [128, 4, 2]) for i in range(2)]
                    rs4 = [sb(a2, "rs4%d" % i, [128, 4]) for i in range(2)]
                    ssq = [sb(a2, "ssq%d" % i, [128, 4]) for i in range(2)]
                    import os
                    for tt in range(16):
                        i2 = tt % 2
                        pu, pv, pg, pt = ps[2 + i2 * 2], ps[3 + i2 * 2], ps[6], ps[7]
                        u_, v_, n_, o_, s_, m_, r_, q_ = ug[i2], vg[i2], vnb[i2], oa[i2], st4[i2], mv4[i2], rs4[i2], ssq[i2]
                        for k in range(8):
                            P.op("pe", lambda e, pu=pu, k=k, tt=tt: e.matmul(pu.t[:], xT.t[:, k, tt * 128:(tt + 1) * 128], wuv.t[:, k, 0:512], start=(k == 0), stop=(k == 7)), xT.b + wuv.b, pu.b)
                        for k in range(8):
                            P.op("pe", lambda e, pv=pv, k=k, tt=tt: e.matmul(pv.t[:], xT.t[:, k, tt * 128:(tt + 1) * 128], wuv.t[:, k, 512:1024], start=(k == 0), stop=(k == 7)), xT.b + wuv.b, pv.b)
                        P.op("act", lambda e, pu=pu, u_=u_: e.activation(out=u_.t[:], in_=pu.t[:], func=AF.Gelu_apprx_tanh), pu.b, u_.b)
                        P.op("act", lambda e, pv=pv, v_=v_: e.activation(out=v_.t[:], in_=pv.t[:], func=AF.Gelu_apprx_tanh), pv.b, v_.b)
                        for g in range(4):
                            P.op("dve", lambda e, g=g, s_=s_, v_=v_: e.bn_stats(out=s_.t[:, g, :], in_=v_.t[:, g * 128:(g + 1) * 128]), v_.b, s_.b)
                        for g in range(4):
                            P.op("dve", lambda e, g=g, s_=s_, m_=m_: e.bn_aggr(out=m_.t[:, g, :], in_=s_.t[:, g, :]), s_.b, m_.b)
                        P.op("dve", lambda e, m_=m_, r_=r_: e.tensor_scalar(out=r_.t[:], in0=m_.t[:, :, 1], scalar1=LN_EPS, scalar2=None, op0=ALU.add), m_.b, r_.b)
                        rsqrt_(r_.t[:], r_.b)
                        for g in range(4):
                            P.op("dve", lambda e, g=g, v_=v_, m_=m_, r_=r_: e.tensor_scalar(out=v_.t[:, g * 128:(g + 1) * 128], in0=v_.t[:, g * 128:(g + 1) * 128], scalar1=m_.t[:, g, 0:1], scalar2=r_.t[:, g:g + 1], op0=ALU.subtract, op1=ALU.mult),
                                 v_.b + m_.b + r_.b, v_.b)
                        P.op("pool", lambda e, v_=v_: e.tensor_tensor(out=v_.t[:], in0=v_.t[:], in1=sg_bc.t[:], op=ALU.mult), v_.b + sg_bc.b, v_.b)
                        P.op("dve", lambda e, v_=v_, n_=n_: e.tensor_tensor(out=n_.t[:], in0=v_.t[:], in1=sb_bc.t[:], op=ALU.add), v_.b + sb_bc.b, n_.b)
                        for g in range(4):
                            P.op("pe", lambda e, g=g, pg=pg, n_=n_: e.matmul(pg.t[:, g * 128:(g + 1) * 128], wcT.t[:, g, :], n_.t[:, g * 128:(g + 1) * 128], start=True, stop=True), wcT.b + n_.b, pg.b)
                        for g in range(4):
                            P.op("dve", lambda e, g=g, pg=pg, o_=o_, u_=u_: e.scalar_tensor_tensor(out=o_.t[:, g * 128:(g + 1) * 128], in0=pg.t[:, g * 128:(g + 1) * 128], scalar=bsp.t[:, g:g + 1], in1=u_.t[:, g * 128:(g + 1) * 128], op0=ALU.add, op1=ALU.mult),
                                 pg.b + bsp.b + u_.b, o_.b)
                        for g in range(4):
                            P.op("dve", lambda e, g=g, o_=o_, q_=q_, i2=i2: e.scalar_tensor_tensor(out=junk[i2].t[:], in0=o_.t[:, g * 128:(g + 1) * 128], scalar=1.0, in1=o_.t[:, g * 128:(g + 1) * 128], op0=ALU.mult, op1=ALU.mult, accum_out=q_.t[:, g:g + 1]), o_.b, q_.b + junk[i2].b)
                        P.op("dve", lambda e, q_=q_: e.tensor_scalar(out=q_.t[:], in0=q_.t[:], scalar1=1.0 / 128.0, scalar2=RMS_EPS, op0=ALU.mult, op1=ALU.add), q_.b, q_.b)
                        rsqrt_(q_.t[:], q_.b)
                        for g in range(4):
                            P.op("dve", lambda e, g=g, o_=o_, q_=q_: e.scalar_tensor_tensor(out=o_.t[:, g * 128:(g + 1) * 128], in0=o_.t[:, g * 128:(g + 1) * 128], scalar=q_.t[:, g:g + 1], in1=gg_bc.t[:, g * 128:(g + 1) * 128], op0=ALU.mult, op1=ALU.mult),
                                 o_.b + q_.b + gg_bc.b, o_.b)
                        for g in range(4):
                            P.op("pe", lambda e, g=g, pt=pt, o_=o_: e.transpose(out=pt.t[:, g * 128:(g + 1) * 128], in_=o_.t[:, g * 128:(g + 1) * 128], identity=ident.t[:]), o_.b + ident.b, pt.b)
                        copy("act", yT.t[:, 0:4, tt * 128:(tt + 1) * 128], pt.t[:].rearrange("p (j t) -> p j t", j=4), pt.b, yT.b)
                    P.barrier()
                if stop in ("a2y", "by"):
                    for c8 in range(8):
                        P.op("dve", lambda e, c8=c8: e.tensor_copy(out=X.t[:, 2 * c8:2 * c8 + 2, :].rearrange("p a b -> p (a b)"), in_=yT.t[:, c8, :]), yT.b + X.b, X.b)
                    P.barrier()
                with contextlib.ExitStack() as cc:
                  if stop not in ("a1", "a1q", "b", "a2", "a2y", "by"):
                    wo_ = sb(cc, "wout", [128, 8, 1024], BF16)
                    wload(wo_, D["w_out"][l], 8, 0, 1024)
                    proj_ln(cc, yT, wo_, l, 0, "c")
                    P.barrier()
            if stop in ("mixer%d" % l, "a1", "a1q", "b", "a2", "a2y", "by"):
                break
            with contextlib.ExitStack() as md:
                kTm = sb(md, "kTm", [128, 8, 256], BF16)
                Vm = sb(md, "Vm", [128, 2, 1024], BF16)
                qmT = sb(md, "qmT", [128, 8, 2048], BF16)
                with contextlib.ExitStack() as d1:
                    wkv = sb(d1, "wkv", [128, 8, 2048], BF16)
                    wload(wkv, D["wkv_mem"][l], 8, 0, 2048)
                    memf = sb(d1, "memf", [128, 2, 1024])
                    memT = sb(d1, "memT", [128, 8, 256], BF16)
                    P.dma("sp", lambda e: e.dma_start(out=memf.t[:], in_=D["mem"].rearrange("(mt p) d -> p mt d", p=128)), writes=memf.b)
                    for mt in range(2):
                        for hf in range(2):
                            pt = ps[hf]
                            for j in range(4):
                                k = hf * 4 + j
                                P.op("pe", lambda e, pt=pt, j=j, k=k, mt=mt: e.transpose(out=pt.t[:, j * 128:(j + 1) * 128], in_=memf.t[:, mt, k * 128:(k + 1) * 128], identity=ident.t[:]), memf.b + ident.b, pt.b)
                            copy(cp_engine(), memT.t[:, hf * 4:(hf + 1) * 4, mt * 128:(mt + 1) * 128], pt.t[:].rearrange("p (j t) -> p j t", j=4), pt.b, memT.b)
                    for c in range(8):
                        pm = ps[2 + c % 4]
                        for k in range(8):
                            P.op("pe", lambda e, pm=pm, c=c, k=k: e.matmul(pm.t[:, 0:256], wkv.t[:, k, c * 128:(c + 1) * 128], memT.t[:, k, :], start=(k == 0), stop=(k == 7)), wkv.b + memT.b, pm.b)
                        copy(cp_engine(), kTm.t[:, c, :], pm.t[:, 0:256], pm.b, kTm.b)
                    for mt in range(2):
                        for dh in range(2):
                            pm = ps[2 + (mt * 2 + dh) % 4]
                            for k in range(8):
                                P.op("pe", lambda e, pm=pm, mt=mt, dh=dh, k=k: e.matmul(pm.t[:], memT.t[:, k, mt * 128:(mt + 1) * 128], wkv.t[:, k, 1024 + dh * 512:1024 + (dh + 1) * 512], start=(k == 0), stop=(k == 7)), wkv.b + memT.b, pm.b)
                            copy(cp_engine(), Vm.t[:, mt, dh * 512:(dh + 1) * 512], pm.t[:], pm.b, Vm.b)
                    P.barrier()
                with contextlib.ExitStack() as d2:
                    xT = sb(d2, "xT3", [128, 8, 2048], BF16)
                    wq = sb(d2, "wq", [128, 8, 1024], BF16)
                    wload(wq, D["wq_mem"][l], 8, 0, 1024)
                    build_xT(xT)
                    for c in range(8):
                        for tb in range(4):
                            pm = ps[2 + (c * 4 + tb) % 4]
                            for k in range(8):
                                P.op("pe", lambda e, pm=pm, c=c, tb=tb, k=k: e.matmul(pm.t[:], wq.t[:, k, c * 128:(c + 1) * 128], xT.t[:, k, tb * 512:(tb + 1) * 512], start=(k == 0), stop=(k == 7)), wq.b + xT.b, pm.b)
                            copy(cp_engine(), qmT.t[:, c, tb * 512:(tb + 1) * 512], pm.t[:], pm.b, qmT.b)
                    P.barrier()
                with contextlib.ExitStack() as d3:
                    OT = sb(d3, "OT", [128, 8, 2048], BF16)
                    wo_ = sb(d3, "womem", [128, 8, 1024], BF16)
                    wload(wo_, D["wo_mem"][l], 8, 0, 1024)
                    PT = [sb(d3, "PT%d" % i, [128, 2, 512], BF16) for i in range(2)]
                    rinv = [sb(d3, "rinv%d" % i, [128, 512]) for i in range(2)]
                    un = 0
                    for h in range(4):
                        for tb in range(4):
                            p_, ri_ = PT[un % 2], rinv[un % 2]
                            un += 1
                            for mt in range(2):
                                sp_ = ps[mt]
                                for c2 in range(2):
                                    P.op("pe", lambda e, sp_=sp_, mt=mt, c2=c2, h=h, tb=tb: e.matmul(sp_.t[:], kTm.t[:, 2 * h + c2, mt * 128:(mt + 1) * 128], qmT.t[:, 2 * h + c2, tb * 512:(tb + 1) * 512], start=(c2 == 0), stop=(c2 == 1)), kTm.b + qmT.b, sp_.b)
                                P.op("act", lambda e, sp_=sp_, p_=p_, mt=mt: e.activation(out=p_.t[:, mt, :], in_=sp_.t[:], func=AF.Exp, scale=1.0 / 16.0), sp_.b, p_.b)
                            sm = ps[2 + un % 2]
                            for mt in range(2):
                                P.op("pe", lambda e, sm=sm, p_=p_, mt=mt: e.matmul(sm.t[:], ones_b.t[:], p_.t[:, mt, :], start=(mt == 0), stop=(mt == 1)), p_.b + ones_b.b, sm.b)
                            P.op("dve", lambda e, sm=sm, ri_=ri_: e.reciprocal(out=ri_.t[:], in_=sm.t[:]), sm.b, ri_.b)
                            for c2 in range(2):
                                po = ps[4 + (un * 2 + c2) % 4]
                                for mt in range(2):
                                    P.op("pe", lambda e, po=po, p_=p_, mt=mt, h=h, c2=c2: e.matmul(po.t[:], Vm.t[:, mt, h * 256 + c2 * 128:h * 256 + (c2 + 1) * 128], p_.t[:, mt, :], start=(mt == 0), stop=(mt == 1)), p_.b + Vm.b, po.b)
                                P.op("dve", lambda e, po=po, ri_=ri_, h=h, c2=c2, tb=tb: e.tensor_tensor(out=OT.t[:, 2 * h + c2, tb * 512:(tb + 1) * 512], in0=po.t[:], in1=ri_.t[:], op=ALU.mult), po.b + ri_.b, OT.b)
                    proj_ln(d3, OT, wo_, l, 1, "d")
                    P.barrier()
            if stop == "mem%d" % l:
                break
            with contextlib.ExitStack() as me:
                Xb = sb(me, "Xb", [128, 16, 1024], BF16)
                POS = sb(me, "POS", [128, 16, 32])
                Ga = sb(me, "Ga", [128, 16, 32])
                bgu = sb(me, "bgu", [128, 16, 32])
                with contextlib.ExitStack() as e0:
                    wr = sb(e0, "wr", [128, 8, 32])
                    P.dma("sp", lambda e: e.dma_start(out=wr.t[:], in_=D["w_router"][l].rearrange("(k p) n -> p k n", p=128)), writes=wr.b)
                    wrh = sb(e0, "wrh", [128, 8, 32], BF16)
                    wrl = sb(e0, "wrl", [128, 8, 32], BF16)
                    P.op("dve", lambda e: e.tensor_copy(out=wrh.t[:], in_=wr.t[:]), wr.b, wrh.b)
                    P.op("dve", lambda e: e.tensor_tensor(out=wrl.t[:], in0=wr.t[:], in1=wrh.t[:], op=ALU.subtract), wr.b + wrh.b, wrl.b)
                    br_bc = sb(e0, "br_bc", [128, 32])
                    bcast_load(br_bc, D["b_router"][l:l + 1, :], 32)
                    bgun = sb(e0, "bgun", [128, 2048])
                    P.op("pool", lambda e: e.memset(bgun.t[:], 0.0), writes=bgun.b)
                    P.dma("sp", lambda e: e.dma_start(out=bgun.t[0:32, :], in_=D["b_gu"][l]), writes=bgun.b)
                    bdn = sb(e0, "bdn", [128, 1024])
                    P.op("pool", lambda e: e.memset(bdn.t[:], 0.0), writes=bdn.b)
                    P.dma("sp", lambda e: e.dma_start(out=bdn.t[0:32, :], in_=D["b_down"][l]), writes=bdn.b)
                    bdnb = sb(e0, "bdnb", [128, 1024], BF16)
                    P.op("dve", lambda e: e.tensor_copy(out=bdnb.t[:], in_=bdn.t[:]), bdn.b, bdnb.b)
                    for c in range(16):
                        pq = ps[4 + c % 4]
                        P.op("pe", lambda e, c=c, pq=pq: e.transpose(out=pq.t[:, 0:128], in_=bgun.t[:, c * 128:(c + 1) * 128], identity=ident.t[:]), bgun.b + ident.b, pq.b)
                        P.op("dve", lambda e, c=c, pq=pq: e.tensor_copy(out=bgu.t[:, c, :], in_=pq.t[:, 0:32]), pq.b, bgu.b)
                    Gp = [sb(e0, "Gp%d" % i, [128, 128]) for i in range(2)]
                    for i in range(2):
                        P.op("pool", lambda e, i=i: e.memset(Gp[i].t[:], 0.0), writes=Gp[i].b)
                    maskb = sb(e0, "maskb", [128, 16, 32], BF16, nb=16)
                    xTt = [sb(e0, "xTt%d" % i, [128, 8, 128], BF16) for i in range(2)]
                    xTf = [sb(e0, "xTf%d" % i, [128, 8, 128], BF16) for i in range(2)]
                    lg = [sb(e0, "lg%d" % i, [128, 32]) for i in range(2)]
                    t8 = [sb(e0, "t8%d" % i, [128, 8]) for i in range(2)]
                    mk = [sb(e0, "mk%d" % i, [128, 32]) for i in range(2)]
                    ex = [sb(e0, "ex%d" % i, [128, 32]) for i in range(2)]
                    nmx = [sb(e0, "nmx%d" % i, [128, 1]) for i in range(2)]
                    ssm = [sb(e0, "ssm%d" % i, [128, 1]) for i in range(2)]
                    GT = [sb(e0, "GT%d" % i, [128, 128], BF16) for i in range(2)]
                    for tt in range(16):
                        i2 = tt % 2
                        xt_, xf = xTt[i2], xTf[i2]
                        for hf in range(2):
                            pt = ps[hf]
                            for j in range(4):
                                k = hf * 4 + j
                                P.op("pe", lambda e, pt=pt, j=j, k=k, tt=tt: e.transpose(out=pt.t[:, j * 128:(j + 1) * 128], in_=X.t[:, tt, k * 128:(k + 1) * 128], identity=ident.t[:]),
                                     [X.b[tt]] + ident.b, pt.b)
                            copy("act", xt_.t[:, hf * 4:(hf + 1) * 4, :], pt.t[:].rearrange("p (j t) -> p j t", j=4), pt.b, xt_.b)
                            P.op("dve", lambda e, xf=xf, xt_=xt_, hf=hf, pt=pt: e.tensor_tensor(out=xf.t[:, hf * 4:(hf + 1) * 4, :], in0=pt.t[:].rearrange("p (j t) -> p j t", j=4), in1=xt_.t[:, hf * 4:(hf + 1) * 4, :], op=ALU.subtract), pt.b + xt_.b, xf.b)
                        P.op("pool", lambda e, tt=tt: e.tensor_copy(out=Xb.t[:, tt, :], in_=X.t[:, tt, :]), [X.b[tt]], Xb.b)
                        pl = ps[2 + i2]
                        l_, t_, m_, e_, n_, s_, g_ = lg[i2], t8[i2], mk[i2], ex[i2], nmx[i2], ssm[i2], GT[i2]
                        for k in range(8):
                            P.op("pe", lambda e, pl=pl, k=k, xt_=xt_: e.matmul(pl.t[:, 0:32], xt_.t[:, k, :], wrh.t[:, k, :], start=(k == 0), stop=False), xt_.b + wrh.b, pl.b)
                            P.op("pe", lambda e, pl=pl, k=k, xt_=xt_: e.matmul(pl.t[:, 0:32], xt_.t[:, k, :], wrl.t[:, k, :], start=False, stop=False), xt_.b + wrl.b, pl.b)
                            P.op("pe", lambda e, pl=pl, xf=xf, k=k: e.matmul(pl.t[:, 0:32], xf.t[:, k, :], wrh.t[:, k, :], start=False, stop=(k == 7)), xf.b + wrh.b, pl.b)
                        P.op("dve", lambda e, pl=pl, l_=l_: e.tensor_tensor(out=l_.t[:], in0=pl.t[:, 0:32], in1=br_bc.t[:], op=ALU.add), pl.b + br_bc.b, l_.b)
                        P.op("dve", lambda e, l_=l_, t_=t_: e.max(out=t_.t[:], in_=l_.t[:]), l_.b, t_.b)
                        P.op("dve", lambda e, l_=l_, t_=t_, m_=m_: e.tensor_scalar(out=m_.t[:], in0=l_.t[:], scalar1=t_.t[:, 3:4], scalar2=None, op0=ALU.is_ge), l_.b + t_.b, m_.b)
                        P.op("dve", lambda e, m_=m_, tt=tt: e.tensor_copy(out=maskb.t[:, tt, :], in_=m_.t[:]), m_.b, [maskb.b[tt]])
                        P.op("dve", lambda e, t_=t_, n_=n_: e.tensor_scalar(out=n_.t[:], in0=t_.t[:, 0:1], scalar1=-1.0, scalar2=None, op0=ALU.mult), t_.b, n_.b)
                        P.op("act", lambda e, l_=l_, e_=e_, n_=n_: e.activation(out=e_.t[:], in_=l_.t[:], func=AF.Exp, bias=n_.t[:, 0:1]), l_.b + n_.b, e_.b)
                        P.op("dve", lambda e, e_=e_, m_=m_, s_=s_: e.scalar_tensor_tensor(out=e_.t[:], in0=e_.t[:], scalar=1.0, in1=m_.t[:], op0=ALU.mult, op1=ALU.mult, accum_out=s_.t[:]), e_.b + m_.b, e_.b + s_.b)
                        P.op("dve", lambda e, s_=s_: e.reciprocal(out=s_.t[:], in_=s_.t[:]), s_.b, s_.b)
                        gp_ = Gp[i2]
                        P.op("dve", lambda e, e_=e_, s_=s_, gp_=gp_: e.tensor_scalar(out=gp_.t[:, 0:32], in0=e_.t[:], scalar1=s_.t[:, 0:1], scalar2=None, op0=ALU.mult), e_.b + s_.b + gp_.b, gp_.b)
                        P.op("dve", lambda e, tt=tt, gp_=gp_: e.tensor_scalar(out=Ga.t[:, tt, :], in0=gp_.t[:, 0:32], scalar1=1.0 / SW_A, scalar2=None, op0=ALU.mult), gp_.b, Ga.b)
                        pp = ps[6 + i2]
                        g0 = (tt // 4) * 4
                        for t2 in range(g0, tt + 1):
                            P.op("pe", lambda e, pp=pp, t2=t2, tt=tt, g0=g0: e.matmul(pp.t[:, 0:32], (umat if t2 == tt else ones_b).t[:], maskb.t[:, t2, :], start=(t2 == g0), stop=(t2 == tt)),
                                 [maskb.b[t2]] + umat.b + ones_b.b, pp.b)
                        P.op("dve", lambda e, pp=pp, m_=m_, tt=tt: e.scalar_tensor_tensor(out=POS.t[:, tt, :], in0=pp.t[:, 0:32], scalar=1.0, in1=m_.t[:], op0=ALU.add, op1=ALU.mult), pp.b + m_.b, POS.b)
                        P.op("dve", lambda e, tt=tt: e.tensor_scalar(out=POS.t[:, tt, :], in0=POS.t[:, tt, :], scalar1=-1.0, scalar2=None, op0=ALU.add), POS.b, POS.b)
                        pg_ = ps[4 + i2]
                        P.op("pe", lambda e, pg_=pg_, gp_=gp_: e.transpose(out=pg_.t[:, 0:128], in_=gp_.t[:], identity=ident.t[:]), gp_.b + ident.b, pg_.b)
                        P.op("dve", lambda e, pg_=pg_, g_=g_: e.tensor_copy(out=g_.t[:], in_=pg_.t[:, 0:128]), pg_.b, g_.b)
                        for dh in range(2):
                            pb_ = ps[4 + i2] if dh == 0 else ps[2 + i2]
                            P.op("pe", lambda e, pb_=pb_, g_=g_, dh=dh: e.matmul(pb_.t[:], g_.t[:], bdnb.t[:, dh * 512:(dh + 1) * 512], start=True, stop=True), g_.b + bdnb.b, pb_.b)
                            P.op("dve", lambda e, pb_=pb_, tt=tt, dh=dh: e.scalar_tensor_tensor(out=X.t[:, tt, dh * 512:(dh + 1) * 512], in0=X.t[:, tt, dh * 512:(dh + 1) * 512], scalar=ALPHA, in1=pb_.t[:], op0=ALU.mult, op1=ALU.add),
                                 [X.b[tt]] + pb_.b, [X.b[tt]])
                    P.barrier()
                with contextlib.ExitStack() as e2:
                  if stop != "e0":
                    NW = 2
                    Wg = [sb(e2, "Wg%d" % i, [128, 8, 512], BF16) for i in range(NW)]
                    Wu = [sb(e2, "Wu%d" % i, [128, 8, 512], BF16) for i in range(NW)]
                    Wd = [sb(e2, "Wd%d" % i, [128, 4, 1024], BF16) for i in range(NW)]
                    gc = [sb(e2, "gc%d" % i, [128, 512]) for i in range(2)]
                    sl = [sb(e2, "sl%d" % i, [128, 512], BF16) for i in range(2)]
                    uc = [sb(e2, "uc%d" % i, [128, 512]) for i in range(2)]
                    AT = sb(e2, "AT", [128, 4, 512], BF16, nb=4)
                    Sel = [sb(e2, "Sel%d" % i, [128, 4, 128], BF16) for i in range(1)] * 2
                    Selg = [sb(e2, "Selg%d" % i, [128, 4, 128]) for i in range(1)] * 2
                    SelT2 = [sb(e2, "SelT%d" % i, [128, 16, 128], BF16, nb=4) for i in range(2)]
                    xeT = sb(e2, "xeT", [128, 8, 512], BF16, nb=4)
                    Yb = [sb(e2, "Yb%d" % i, [128, 4, 1024], BF16) for i in range(2)]
                    units = [(e_, hf) for e_ in range(n_experts) for hf in range(2)]

                    def load_unit(ui):
                        e_, hf = units[ui]
                        wi = ui % NW
                        gv = D["w_gu"][l, e_].rearrange("(k p) n -> p k n", p=128)
                        dv = D["w_down"][l, e_, hf * 512:(hf + 1) * 512, :].rearrange("(j p) n -> p j n", p=128)
                        for k0 in (0, 4):
                            P.dma("pool", lambda e, k0=k0: e.dma_start(out=Wg[wi].t[:, k0:k0 + 4, :], in_=gv[:, k0:k0 + 4, hf * 512:(hf + 1) * 512]), writes=Wg[wi].b)
                        for k0 in (0, 4):
                            P.dma("pool", lambda e, k0=k0: e.dma_start(out=Wu[wi].t[:, k0:k0 + 4, :], in_=gv[:, k0:k0 + 4, 1024 + hf * 512:1024 + (hf + 1) * 512]), writes=Wu[wi].b)
                        for j0 in (0, 2):
                            P.dma("pool", lambda e, j0=j0: e.dma_start(out=Wd[wi].t[:, j0:j0 + 2, :], in_=dv[:, j0:j0 + 2, :]), writes=Wd[wi].b)

                    load_unit(0)
                    load_unit(1)
                    cnt = 0
                    def scatter(e_):
                        SelT = SelT2[e_ % 2]
                        for tt in range(16):
                            grp = tt // 4
                            for dh in range(2):
                                py = ps[(tt * 2 + dh) % 4]
                                for hf in range(2):
                                    P.op("pe", lambda e, py=py, tt=tt, grp=grp, dh=dh, hf=hf: e.matmul(py.t[:], SelT.t[:, tt, :], Yb[hf].t[:, grp, dh * 512:(dh + 1) * 512], start=(hf == 0), stop=(hf == 1)), [SelT.b[grp]] + Yb[hf].b, py.b)
                                P.op("dve", lambda e, py=py, tt=tt, dh=dh: e.tensor_tensor(out=X.t[:, tt, dh * 512:(dh + 1) * 512], in0=py.t[:], in1=X.t[:, tt, dh * 512:(dh + 1) * 512], op=ALU.add),
                                     py.b + [X.b[tt]], [X.b[tt]])

                    for e_ in range(n_experts):
                        SelT = SelT2[e_ % 2]
                        for grp in range(4):
                            s_, sg_ = Sel[grp % 2], Selg[grp % 2]
                            for t4 in range(4):
                                tt = grp * 4 + t4
                                P.op("dve", lambda e, s_=s_, t4=t4, tt=tt, e_=e_: e.tensor_scalar(out=s_.t[:, t4, :], in0=iosl.t[:], scalar1=POS.t[:, tt, e_:e_ + 1], scalar2=None, op0=ALU.is_equal), iosl.b + POS.b, s_.b)
                                P.op("dve", lambda e, sg_=sg_, t4=t4, tt=tt, e_=e_: e.tensor_scalar(out=sg_.t[:, t4, :], in0=iosl.t[:], scalar1=POS.t[:, tt, e_:e_ + 1], scalar2=Ga.t[:, tt, e_:e_ + 1], op0=ALU.is_equal, op1=ALU.mult), iosl.b + POS.b + Ga.b, sg_.b)
                            for kh in range(2):
                                pgt = ps[kh]
                                for j in range(4):
                                    k = kh * 4 + j
                                    for t4 in range(4):
                                        tt = grp * 4 + t4
                                        P.op("pe", lambda e, pgt=pgt, j=j, k=k, t4=t4, tt=tt, s_=s_: e.matmul(pgt.t[:, j * 128:(j + 1) * 128], Xb.t[:, tt, k * 128:(k + 1) * 128], s_.t[:, t4, :], start=(t4 == 0), stop=(t4 == 3)),
                                             Xb.b + s_.b, pgt.b)
                                copy("act", xeT.t[:, kh * 4:(kh + 1) * 4, grp * 128:(grp + 1) * 128], pgt.t[:].rearrange("p (j t) -> p j t", j=4), pgt.b, [xeT.b[grp]])
                            ptr = ps[2 + grp % 2]
                            for t4 in range(4):
                                P.op("pe", lambda e, ptr=ptr, t4=t4, sg_=sg_: e.transpose(out=ptr.t[:, t4 * 128:(t4 + 1) * 128], in_=sg_.t[:, t4, :], identity=ident.t[:]), sg_.b + ident.b, ptr.b)
                            copy("act", SelT.t[:, grp * 4:(grp + 1) * 4, :], ptr.t[:].rearrange("p (j t) -> p j t", j=4), ptr.b, [SelT.b[grp]])
                        if e_ > 0:
                            scatter(e_ - 1)
                        for hf in range(2):
                            ui = e_ * 2 + hf
                            wi = ui % NW
                            yb_ = Yb[hf]
                            for j in range(4):
                                i2 = cnt % 2
                                cnt += 1
                                pg, pu = ps[4 + i2 * 2], ps[5 + i2 * 2]
                                g_, sl_, u_ = gc[i2], sl[i2], uc[i2]
                                cg = hf * 4 + j
                                cu = 8 + hf * 4 + j
                                for k in range(8):
                                    P.op("pe", lambda e, pg=pg, k=k, j=j, wi=wi: e.matmul(pg.t[:], Wg[wi].t[:, k, j * 128:(j + 1) * 128], xeT.t[:, k, :], start=(k == 0), stop=(k == 7)), Wg[wi].b + xeT.b, pg.b)
                                for k in range(8):
                                    P.op("pe", lambda e, pu=pu, k=k, j=j, wi=wi: e.matmul(pu.t[:], Wu[wi].t[:, k, j * 128:(j + 1) * 128], xeT.t[:, k, :], start=(k == 0), stop=(k == 7)), Wu[wi].b + xeT.b, pu.b)
                                P.op("dve", lambda e, pg=pg, g_=g_, cg=cg, e_=e_: e.tensor_scalar(out=g_.t[:], in0=pg.t[:], scalar1=bgu.t[:, cg, e_:e_ + 1], scalar2=SW_L, op0=ALU.add, op1=ALU.min), pg.b + bgu.b, g_.b)
                                P.op("act", lambda e, g_=g_, sl_=sl_: e.activation(out=sl_.t[:], in_=g_.t[:], func=AF.Silu, scale=SW_A), g_.b, sl_.b)
                                P.op("dve", lambda e, pu=pu, u_=u_, cu=cu, e_=e_: e.tensor_scalar(out=u_.t[:], in0=pu.t[:], scalar1=bgu.t[:, cu, e_:e_ + 1], scalar2=SW_L, op0=ALU.add, op1=ALU.min), pu.b + bgu.b, u_.b)
                                P.op("dve", lambda e, u_=u_: e.tensor_scalar(out=u_.t[:], in0=u_.t[:], scalar1=-SW_L, scalar2=1.0, op0=ALU.max, op1=ALU.add), u_.b, u_.b)
                                P.op("dve", lambda e, sl_=sl_, u_=u_, j=j: e.tensor_tensor(out=AT.t[:, j, :], in0=sl_.t[:], in1=u_.t[:], op=ALU.mult), sl_.b + u_.b, [AT.b[j]])
                            for s4 in range(4):
                                for dh in range(2):
                                    py = ps[(s4 * 2 + dh) % 4]
                                    for j in range(4):
                                        P.op("pe", lambda e, py=py, j=j, s4=s4, dh=dh, wi=wi: e.matmul(py.t[:], AT.t[:, j, s4 * 128:(s4 + 1) * 128], Wd[wi].t[:, j, dh * 512:(dh + 1) * 512], start=(j == 0), stop=(j == 3)), [AT.b[j]] + Wd[wi].b, py.b)
                                    copy("act", yb_.t[:, s4, dh * 512:(dh + 1) * 512], py.t[:], py.b, yb_.b)
                            if ui + 2 < len(units):
                                load_unit(ui + 2)
                    scatter(n_experts - 1)
                    P.barrier()
                with contextlib.ExitStack() as e3:
                  if stop not in ("e0", "e2"):
                    gbc = sb(e3, "gbce", [128, 1024]); bbc = sb(e3, "bbce", [128, 1024])
                    bcast_load(gbc, D["ln_g"][l, 2:3, :], 1024)
                    bcast_load(bbc, D["ln_b"][l, 2:3, :], 1024)
                    tmps = ln_tmps(e3, "e")
                    for tt in range(16):
                        ln_tile(e3, X.t[:, tt, :], [X.b[tt]], tt, gbc, bbc, tmps[tt % 2])
                    P.barrier()
            if stop in ("moe%d" % l, "e0", "e2"):
                break

        ov = out_d.rearrange("(tt p) d -> p tt d", p=128)
        for q4 in range(4):
            P.dma("sp", lambda e, q4=q4: e.dma_start(out=ov[:, q4 * 4:(q4 + 1) * 4, :], in_=X.t[:, q4 * 4:(q4 + 1) * 4, :]),
                  reads=X.b[q4 * 4:(q4 + 1) * 4], writes=[Buf()], is_output=True)
        with nc.allow_non_contiguous_dma(reason="small parameter loads"):
            P.emit()
    return nc


def kernel(**inputs):
    n = 8
    nc = build()
    x = np.ascontiguousarray(np.asarray(inputs["x"], dtype=np.float32))
    mem = np.ascontiguousarray(np.asarray(inputs["mem"], dtype=np.float32))
    ws = {k: np.ascontiguousarray(np.asarray(inputs[k], dtype=np.float32)) for k in W_NAMES}
    in_maps = []
    for b in range(n):
        m = {"x": x[b], "mem": mem[b]}
        m.update(ws)
        in_maps.append(m)
    res = run_bass_kernel_spmd(nc, in_maps, core_ids=list(range(n)))
    return np.stack([np.asarray(r["out"]) for r in res.results], axis=0).astype(np.float32)
```
